# Optimizing a Trainium2 kernel written in Bass

```python
import math
import jax, jax.numpy as jnp
from jax import lax
import numpy as np

D_MODEL = 2048
BATCH = 2
SEQ = 8192
DEPTH = 2

NORM_EPS = 1e-6
ATT_HEADS = 16
QK_NOPE = 128
QK_ROPE = 64
V_DIM = 128
KV_RANK = 512
ROPE_THETA = 10000.0
Q_BLOCK = 128
SSM_HEADS = 32
SSM_HEAD_DIM = 64
SSM_INNER = SSM_HEADS * SSM_HEAD_DIM
SSM_GROUPS = 4
SSM_STATE = 128
SSM_CONV = 5
SSM_CHUNK = 128
SCONV_DIM = D_MODEL
SCONV_K = 3
N_GROUPS = 8
EXPERTS_PER_GROUP = 4
N_EXPERTS = N_GROUPS * EXPERTS_PER_GROUP
TOP_K = 2
D_EXPERT = 512
N_BRANCH = 3
Q_DIM = ATT_HEADS * (QK_NOPE + QK_ROPE)
ATT_OUT = ATT_HEADS * V_DIM
XBC_DIM = SSM_INNER + 2 * SSM_GROUPS * SSM_STATE
SPLIT_SIZES = (Q_DIM, KV_RANK, QK_ROPE, SSM_INNER, XBC_DIM, 2 * SSM_HEADS,
               SCONV_DIM, SCONV_DIM, SCONV_DIM, N_BRANCH * D_MODEL)
IN_COLS = (Q_DIM + KV_RANK + QK_ROPE + SSM_INNER + XBC_DIM + 2 * SSM_HEADS
           + 3 * SCONV_DIM + N_BRANCH * D_MODEL)
BRANCH_IN = ATT_OUT + SSM_INNER + SCONV_DIM

kernel_name = "hybrid_mla_ssd_shortconv_hiermoe_encoder"


def rms_norm(x, w):
    xf = x.astype(jnp.float32)
    y = xf * lax.rsqrt(jnp.mean(xf * xf, axis=-1, keepdims=True) + NORM_EPS)
    return (y * w.astype(jnp.float32)).astype(x.dtype)


def split_cols(u):
    offsets = [int(o) for o in np.cumsum(SPLIT_SIZES)[:-1]]
    return jnp.split(u, offsets, axis=-1)


def depthwise_conv(x, w, pad):
    c = w.shape[-1]
    return lax.conv_general_dilated(
        x, w[:, None, :].astype(x.dtype), window_strides=(1,), padding=[(pad, pad)],
        dimension_numbers=('NWC', 'WIO', 'NWC'), feature_group_count=c)


def apply_rope(t, cos, sin):
    t1, t2 = jnp.split(t, 2, axis=-1)
    return jnp.concatenate([t1 * cos - t2 * sin, t1 * sin + t2 * cos], axis=-1)


def mla_attention(q, c_kv, k_rope, positions, kv_norm_w, w_uk, w_uv):
    b, s, _ = q.shape
    q = q.reshape(b, s, ATT_HEADS, QK_NOPE + QK_ROPE)
    q_nope, q_rope = q[..., :QK_NOPE], q[..., QK_NOPE:]
    c_kv = rms_norm(c_kv, kv_norm_w)
    k_nope = (c_kv @ w_uk).reshape(b, s, ATT_HEADS, QK_NOPE)
    v = (c_kv @ w_uv).reshape(b, s, ATT_HEADS, V_DIM)
    freqs = ROPE_THETA ** (-jnp.arange(0, QK_ROPE, 2, dtype=jnp.float32) / QK_ROPE)
    ang = positions.astype(jnp.float32)[..., None] * freqs
    cos, sin = jnp.cos(ang), jnp.sin(ang)
    q_rope = apply_rope(q_rope, cos[:, :, None, :], sin[:, :, None, :]).astype(q.dtype)
    k_rope = apply_rope(k_rope, cos, sin).astype(q.dtype)
    scale = (QK_NOPE + QK_ROPE) ** -0.5
    nblk = s // Q_BLOCK
    qn = jnp.moveaxis(q_nope.reshape(b, nblk, Q_BLOCK, ATT_HEADS, QK_NOPE), 1, 0)
    qr = jnp.moveaxis(q_rope.reshape(b, nblk, Q_BLOCK, ATT_HEADS, QK_ROPE), 1, 0)

    def block(args):
        qn_b, qr_b = args
        logits = (jnp.einsum('bqhd,bkhd->bhqk', qn_b, k_nope)
                  + jnp.einsum('bqhr,bkr->bhqk', qr_b, k_rope))
        p = jax.nn.softmax(logits.astype(jnp.float32) * scale, axis=-1)
        return jnp.einsum('bhqk,bkhv->bqhv', p.astype(v.dtype), v)

    out = lax.map(block, (qn, qr))
    return jnp.moveaxis(out, 0, 1).reshape(b, s, ATT_OUT)


def segsum(a):
    cs = jnp.cumsum(a, axis=-1)
    diff = cs[..., :, None] - cs[..., None, :]
    q = a.shape[-1]
    mask = jnp.tril(jnp.ones((q, q), dtype=bool))
    return jnp.where(mask, diff, -jnp.inf)


def ssd_scan(x, dt, A, B, C):
    b, s = x.shape[:2]
    nc = s // SSM_CHUNK
    r = SSM_HEADS // SSM_GROUPS
    xdt = (x * dt[..., None]).reshape(b, nc, SSM_CHUNK, SSM_GROUPS, r, SSM_HEAD_DIM)
    a = (dt * A).reshape(b, nc, SSM_CHUNK, SSM_GROUPS, r).transpose(0, 1, 3, 4, 2)
    Bc = B.reshape(b, nc, SSM_CHUNK, SSM_GROUPS, SSM_STATE)
    Cc = C.reshape(b, nc, SSM_CHUNK, SSM_GROUPS, SSM_STATE)
    a_cum = jnp.cumsum(a, axis=-1)
    decay_in = jnp.exp(segsum(a))
    cb = jnp.einsum('bclgn,bcsgn->bcgls', Cc, Bc)
    y_diag = jnp.einsum('bcgrls,bcsgrp->bclgrp', cb[:, :, :, None] * decay_in, xdt)
    decay_states = jnp.exp(a_cum[..., -1:] - a_cum)
    states = jnp.einsum('bclgn,bcgrl,bclgrp->bcgrpn', Bc, decay_states, xdt)
    chunk_decay = jnp.exp(a_cum[..., -1])

    def step(h, inp):
        st, dec = inp
        return dec[..., None, None] * h + st, h

    h0 = jnp.zeros_like(states[:, 0])
    _, prev = lax.scan(step, h0, (jnp.moveaxis(states, 1, 0), jnp.moveaxis(chunk_decay, 1, 0)))
    prev = jnp.moveaxis(prev, 0, 1)
    y_off = jnp.einsum('bclgn,bcgrpn,bcgrl->bclgrp', Cc, prev, jnp.exp(a_cum))
    return (y_diag + y_off).reshape(b, s, SSM_HEADS, SSM_HEAD_DIM)


def ssd_mixer(z, xbc, dt_raw, conv_w, conv_b, A_log, dt_bias, D_skip, norm_w):
    b, s, _ = z.shape
    f32 = jnp.float32
    xbc = jax.nn.silu(depthwise_conv(xbc, conv_w, SSM_CONV // 2) + conv_b)
    xs, Bm, Cm = jnp.split(xbc, [SSM_INNER, SSM_INNER + SSM_GROUPS * SSM_STATE], axis=-1)
    xs = xs.reshape(b, s, SSM_HEADS, SSM_HEAD_DIM).astype(f32)
    Bm = Bm.reshape(b, s, SSM_GROUPS, SSM_STATE).astype(f32)
    Cm = Cm.reshape(b, s, SSM_GROUPS, SSM_STATE).astype(f32)
    dt = jax.nn.softplus(dt_raw.astype(f32).reshape(b, s, 2, SSM_HEADS) + dt_bias.astype(f32))
    A = -jnp.exp(A_log.astype(f32))
    y_fwd = ssd_scan(xs, dt[:, :, 0], A[0], Bm, Cm)
    flip = lambda t: jnp.flip(t, axis=1)
    y_bwd = flip(ssd_scan(flip(xs), flip(dt[:, :, 1]), A[1], flip(Bm), flip(Cm)))
    y = y_fwd + y_bwd + D_skip.astype(f32)[:, None] * xs
    y = y.reshape(b, s, SSM_INNER) * jax.nn.silu(z.astype(f32))
    yg = y.reshape(b, s, SSM_GROUPS, SSM_INNER // SSM_GROUPS)
    yg = yg * lax.rsqrt(jnp.mean(yg * yg, axis=-1, keepdims=True) + NORM_EPS)
    return (yg.reshape(b, s, SSM_INNER) * norm_w.astype(f32)).astype(z.dtype)


def short_gated_conv(b_gate, c_gate, xs, w):
    return b_gate * depthwise_conv(c_gate * xs, w, SCONV_K // 2)


def hybrid_mixer(n, positions, w_in, kv_norm_w, w_uk, w_uv, ssm_conv_w, ssm_conv_b,
                 ssm_A_log, ssm_dt_bias, ssm_D, ssm_norm_w, sconv_w, w_branch, w_o):
    b, s, _ = n.shape
    u = n @ w_in
    q, c_kv, k_rope, z, xbc, dt_raw, sc_b, sc_c, sc_x, gate_logits = split_cols(u)
    y_att = mla_attention(q, c_kv, k_rope, positions, kv_norm_w, w_uk, w_uv)
    y_ssm = ssd_mixer(z, xbc, dt_raw, ssm_conv_w, ssm_conv_b, ssm_A_log, ssm_dt_bias, ssm_D, ssm_norm_w)
    y_conv = short_gated_conv(sc_b, sc_c, sc_x, sconv_w)
    p_att, p_ssm, p_conv = jnp.split(w_branch, [ATT_OUT, ATT_OUT + SSM_INNER], axis=0)
    g = jax.nn.sigmoid(gate_logits.astype(jnp.float32)).reshape(b, s, N_BRANCH, D_MODEL)
    merged = (g[:, :, 0] * (y_att @ p_att) + g[:, :, 1] * (y_ssm @ p_ssm)
              + g[:, :, 2] * (y_conv @ p_conv))
    return merged.astype(n.dtype) @ w_o


def hierarchical_moe(n, rg_w, rg_b, re_w, re_b, w_gate, w_up, w_down):
    b, s, d = n.shape
    f32 = jnp.float32
    t = n.reshape(b * s, d)
    g_logits = (t @ rg_w).astype(f32) + rg_b.astype(f32)
    g_prob = jax.nn.softmax(g_logits, axis=-1)
    g_idx = jnp.argmax(g_logits, axis=-1)
    g_w = jnp.take_along_axis(g_prob, g_idx[:, None], axis=-1)
    e_logits = ((t @ re_w).astype(f32) + re_b.astype(f32)).reshape(-1, N_GROUPS, EXPERTS_PER_GROUP)
    e_in_group = jnp.take_along_axis(e_logits, g_idx[:, None, None], axis=1)[:, 0]
    top_val, top_idx = lax.top_k(e_in_group, TOP_K)
    top_w = jax.nn.softmax(top_val, axis=-1) * g_w
    expert_id = g_idx[:, None] * EXPERTS_PER_GROUP + top_idx
    combine = jnp.sum(jax.nn.one_hot(expert_id, N_EXPERTS, dtype=f32) * top_w[..., None], axis=1)
    out = jnp.zeros((b * s, d), f32)
    for e in range(N_EXPERTS):
        ye = (jax.nn.silu(t @ w_gate[e]) * (t @ w_up[e])) @ w_down[e]
        out = out + combine[:, e:e + 1] * ye
    return out.reshape(b, s, d).astype(n.dtype)


def setup_inputs(seed: int = 0) -> dict:
    key = jax.random.key(seed)
    ks = jax.random.split(key, 25)
    f32 = jnp.float32
    L = DEPTH

    def nrm(k, shape, scale):
        return jax.random.normal(k, shape, f32) * scale

    def gain(k, shape):
        return 1.0 + 0.02 * jax.random.normal(k, shape, f32)

    x = nrm(ks[0], (BATCH, SEQ, D_MODEL), 1.0)
    positions = (jnp.arange(SEQ, dtype=jnp.int32)[None, :]
                 + jax.random.randint(ks[1], (BATCH, 1), 0, 1024, dtype=jnp.int32))
    dt0 = jnp.exp(jax.random.uniform(ks[10], (L, 2, SSM_HEADS), f32,
                                     minval=math.log(1e-3), maxval=math.log(1e-1)))
    return {
        "x": x,
        "positions": positions,
        "attn_norm_w": gain(ks[2], (L, D_MODEL)),
        "w_in": nrm(ks[3], (L, D_MODEL, IN_COLS), D_MODEL ** -0.5),
        "kv_norm_w": gain(ks[4], (L, KV_RANK)),
        "w_uk": nrm(ks[5], (L, KV_RANK, ATT_HEADS * QK_NOPE), KV_RANK ** -0.5),
        "w_uv": nrm(ks[6], (L, KV_RANK, ATT_HEADS * V_DIM), KV_RANK ** -0.5),
        "ssm_conv_w": nrm(ks[7], (L, SSM_CONV, XBC_DIM), SSM_CONV ** -0.5),
        "ssm_conv_b": nrm(ks[8], (L, XBC_DIM), 0.02),
        "ssm_A_log": jnp.log(jax.random.uniform(ks[9], (L, 2, SSM_HEADS), f32, minval=1.0, maxval=16.0)),
        "ssm_dt_bias": dt0 + jnp.log(-jnp.expm1(-dt0)),
        "ssm_D": gain(ks[11], (L, SSM_HEADS)),
        "ssm_norm_w": gain(ks[12], (L, SSM_INNER)),
        "sconv_w": nrm(ks[13], (L, SCONV_K, SCONV_DIM), SCONV_K ** -0.5),
        "w_branch": nrm(ks[14], (L, BRANCH_IN, D_MODEL), D_MODEL ** -0.5),
        "w_o": nrm(ks[15], (L, D_MODEL, D_MODEL), D_MODEL ** -0.5),
        "ffn_norm_w": gain(ks[16], (L, D_MODEL)),
        "router_group_w": nrm(ks[17], (L, D_MODEL, N_GROUPS), D_MODEL ** -0.5),
        "router_group_b": nrm(ks[18], (L, N_GROUPS), 0.01),
        "router_expert_w": nrm(ks[19], (L, D_MODEL, N_EXPERTS), D_MODEL ** -0.5),
        "router_expert_b": nrm(ks[20], (L, N_EXPERTS), 0.01),
        "expert_w_gate": nrm(ks[21], (L, N_EXPERTS, D_MODEL, D_EXPERT), D_MODEL ** -0.5),
        "expert_w_up": nrm(ks[22], (L, N_EXPERTS, D_MODEL, D_EXPERT), D_MODEL ** -0.5),
        "expert_w_down": nrm(ks[23], (L, N_EXPERTS, D_EXPERT, D_MODEL), D_EXPERT ** -0.5),
        "final_norm_w": gain(ks[24], (D_MODEL,)),
    }


def reference(x, positions, attn_norm_w, w_in, kv_norm_w, w_uk, w_uv, ssm_conv_w, ssm_conv_b,
              ssm_A_log, ssm_dt_bias, ssm_D, ssm_norm_w, sconv_w, w_branch, w_o, ffn_norm_w,
              router_group_w, router_group_b, router_expert_w, router_expert_b,
              expert_w_gate, expert_w_up, expert_w_down, final_norm_w):
    h = x
    for l in range(DEPTH):
        h = h + hybrid_mixer(rms_norm(h, attn_norm_w[l]), positions, w_in[l], kv_norm_w[l], w_uk[l],
                             w_uv[l], ssm_conv_w[l], ssm_conv_b[l], ssm_A_log[l], ssm_dt_bias[l],
                             ssm_D[l], ssm_norm_w[l], sconv_w[l], w_branch[l], w_o[l])
        h = h + hierarchical_moe(rms_norm(h, ffn_norm_w[l]), router_group_w[l], router_group_b[l],
                                 router_expert_w[l], router_expert_b[l], expert_w_gate[l],
                                 expert_w_up[l], expert_w_down[l])
    return rms_norm(h, final_norm_w)
```

```python
import os
import numpy as np
from contextlib import ExitStack
import concourse.bass as bass
import concourse.mybir as mybir
from concourse.bass_utils import run_bass_kernel_spmd

F32 = mybir.dt.float32
BF16 = mybir.dt.bfloat16
I32 = mybir.dt.int32
ALU = mybir.AluOpType
AF = mybir.ActivationFunctionType
PE, ACT, DVE, POOL, SP = "pe", "act", "dve", "pool", "sp"

D = 2048
EPS = 1e-6
IN_COLS = 21120
TWO_PI = float(2 * np.pi)


class Buf:
    __slots__ = ("name", "last_w", "readers", "excl")

    def __init__(self, name="", excl=False):
        self.name = name
        self.last_w = None
        self.readers = []
        self.excl = excl


class Op:
    __slots__ = ("eng", "fn", "reads", "writes", "is_dma", "deps", "needs_inc",
                 "inc_val", "dma_sem", "dma_val", "dma_prev")

    def __init__(self, eng, fn, reads, writes, is_dma):
        self.eng = eng
        self.fn = fn
        self.reads = reads
        self.writes = writes
        self.is_dma = is_dma
        self.deps = ()
        self.needs_inc = False
        self.inc_val = 0
        self.dma_sem = None
        self.dma_val = 0
        self.dma_prev = None


class Sched:
    N_DMA_SEMS = 24
    SAME_ENGINE_SYNC = (ACT, DVE, POOL)

    def __init__(self, nc):
        self.nc = nc
        self.ops = []
        self.engs = {PE: nc.tensor, ACT: nc.scalar, DVE: nc.vector, POOL: nc.gpsimd, SP: nc.sync}
        self.store_bufs = []

    def op(self, eng, fn, reads=(), writes=()):
        self.ops.append(Op(eng, fn, tuple(b for b in reads if b is not None),
                           tuple(b for b in writes if b is not None), False))

    def dma(self, eng, fn, reads=(), writes=()):
        r = tuple(b for b in reads if b is not None)
        self.store_bufs.extend(r)
        self.ops.append(Op(eng, fn, r, tuple(b for b in writes if b is not None), True))

    def barrier(self):
        self.store_bufs = []
        self.ops.append(Op(None, None, (), (), False))

    def emit(self, stack):
        nc = self.nc
        ops = self.ops
        last_on = {}
        for i, o in enumerate(ops):
            if o.eng is None:
                for e, li in last_on.items():
                    ops[li].needs_inc = True
                continue
            if not o.is_dma:
                last_on[o.eng] = i
            deps = set()
            for b in o.reads:
                if b.last_w is not None:
                    deps.add(b.last_w)
                if b.excl:
                    deps.update(r for r in b.readers if ops[r].eng != o.eng)
            for b in o.writes:
                if b.last_w is not None:
                    deps.add(b.last_w)
                deps.update(b.readers)
            for b in o.writes:
                b.last_w = i
                b.readers = []
            for b in o.reads:
                if b.last_w != i:
                    b.readers.append(i)
            deps.discard(i)
            o.deps = deps
        for i, o in enumerate(ops):
            if o.eng is None:
                continue
            nd = []
            for d in o.deps:
                p = ops[d]
                if p.is_dma:
                    nd.append(d)
                    continue
                if p.eng == o.eng and not o.is_dma and o.eng not in self.SAME_ENGINE_SYNC:
                    continue
                nd.append(d)
                p.needs_inc = True
            o.deps = nd
        esem = {e: stack.enter_context(nc.semaphore("s_" + e)) for e in self.engs}
        dsems = {e: [stack.enter_context(nc.semaphore("d_%s_%d" % (e, k))) for k in range(self.N_DMA_SEMS)]
                 for e in (SP, POOL)}
        cnt = {e: 0 for e in self.engs}
        dcount = {e: 0 for e in dsems}
        dlast = {e: [None] * self.N_DMA_SEMS for e in dsems}
        dval = {e: [0] * self.N_DMA_SEMS for e in dsems}
        for i, o in enumerate(ops):
            if o.eng is None:
                continue
            if o.is_dma:
                k = dcount[o.eng] % self.N_DMA_SEMS
                dcount[o.eng] += 1
                o.dma_sem = dsems[o.eng][k]
                o.dma_prev = dlast[o.eng][k]
                dval[o.eng][k] += 16
                o.dma_val = dval[o.eng][k]
                dlast[o.eng][k] = i
            elif o.needs_inc:
                cnt[o.eng] += 1
                o.inc_val = cnt[o.eng]
        waited = {e: {} for e in self.engs}
        cur_cnt = {e: 0 for e in self.engs}
        cur_d = {}
        for i, o in enumerate(ops):
            if o.eng is None:
                for f in self.engs:
                    ef = self.engs[f]
                    wf = waited[f]
                    for e in self.engs:
                        if e != f and cur_cnt[e] > wf.get(("e", e), 0):
                            ef.wait_ge(esem[e], cur_cnt[e])
                            wf[("e", e)] = cur_cnt[e]
                    for key, (sem, val) in cur_d.items():
                        if val > wf.get(key, 0):
                            ef.wait_ge(sem, val)
                            wf[key] = val
                continue
            eng = self.engs[o.eng]
            w = waited[o.eng]
            need = {}
            dl = list(o.deps)
            if o.is_dma and o.dma_prev is not None:
                dl.append(o.dma_prev)
            for d in dl:
                p = ops[d]
                if p.is_dma:
                    key = ("d", p.eng, id(p.dma_sem))
                    sem, val = p.dma_sem, p.dma_val
                else:
                    key = ("e", p.eng)
                    sem, val = esem[p.eng], p.inc_val
                if w.get(key, 0) >= val:
                    continue
                if key not in need or need[key][1] < val:
                    need[key] = (sem, val)
            for key, (sem, val) in need.items():
                eng.wait_ge(sem, val)
                w[key] = val
            ins = o.fn()
            if o.is_dma:
                ins.then_inc(o.dma_sem, 16)
                cur_d[("d", o.eng, id(o.dma_sem))] = (o.dma_sem, o.dma_val)
            elif o.needs_inc:
                ins.then_inc(esem[o.eng], 1)
                cur_cnt[o.eng] = o.inc_val
        sp = self.engs[SP]
        for e in dsems:
            for k in range(self.N_DMA_SEMS):
                if dval[e][k] > 0:
                    sp.wait_ge(dsems[e][k], dval[e][k])
        for e in self.engs:
            if e != SP and cnt[e] > 0:
                sp.wait_ge(esem[e], cnt[e])


class TV:
    __slots__ = ("ap", "b")

    def __init__(self, ap, b):
        self.ap = ap
        self.b = b

    def __getitem__(self, k):
        return TV(self.ap[k], self.b)

    def v(self, f):
        return TV(f(self.ap), self.b)

    def re(self, s, **kw):
        return TV(self.ap.rearrange(s, **kw), self.b)

    def bc(self, axis, shape):
        return TV(self.ap.unsqueeze(axis).to_broadcast(shape), self.b)


class KB:
    def __init__(self, nc, st, arena_words=49152):
        self.nc = nc
        self.st = st
        self.S = Sched(nc)
        self.arena = st.enter_context(nc.sbuf_tensor("arena", [128, arena_words], F32))
        self.words = arena_words
        self.off = 0
        self.marks = []
        self.ps = []
        for i in range(8):
            p = st.enter_context(nc.psum_tensor("ps%d" % i, [128, 512], F32))
            self.ps.append(TV(p[:, :], Buf("ps%d" % i, excl=True)))

    def alloc(self, n, dt=F32, name=""):
        w = n if dt in (F32, I32) else (n + 1) // 2
        assert self.off + w <= self.words, "arena overflow %s: %d + %d > %d" % (name, self.off, w, self.words)
        ap = self.arena[:, self.off:self.off + w]
        self.off += w
        if dt != F32:
            ap = ap.bitcast(dt)
        return TV(ap, Buf(name))

    def mark(self):
        self.marks.append(self.off)

    def release(self):
        self.off = self.marks.pop()
        self.S.barrier()

    def dram(self, name, shape, dt, kind="Internal"):
        return TV(self.nc.dram_tensor(name, list(shape), dt, kind=kind).ap(), None)

    def mm(self, out, lhsT, rhs, start=True, stop=True, extra_r=()):
        nc = self.nc
        self.S.op(PE, lambda: nc.tensor.matmul(out.ap, lhsT.ap, rhs.ap, start=start, stop=stop),
                  [lhsT.b, rhs.b] + [x.b for x in extra_r], [out.b])

    def tr(self, out, in_, ident):
        nc = self.nc
        self.S.op(PE, lambda: nc.tensor.transpose(out.ap, in_.ap, ident.ap), [in_.b, ident.b], [out.b])

    def act(self, out, in_, func, bias=None, scale=1.0, accum=None):
        nc = self.nc
        kw = {}
        r = [in_.b]
        w = [out.b]
        if bias is not None:
            kw["bias"] = bias.ap
            r.append(bias.b)
        if accum is not None:
            kw["accum_out"] = accum.ap
            w.append(accum.b)
        self.S.op(ACT, lambda: nc.scalar.activation(out=out.ap, in_=in_.ap, func=func, scale=scale, **kw), r, w)

    def _ve(self, eng):
        return self.nc.vector if eng == DVE else self.nc.gpsimd

    def tt(self, out, a, b, op, eng=DVE):
        e = self._ve(eng)
        self.S.op(eng, lambda: e.tensor_tensor(out=out.ap, in0=a.ap, in1=b.ap, op=op), [a.b, b.b], [out.b])

    def ts(self, out, a, s1, op0, s2=None, op1=None, eng=DVE):
        e = self._ve(eng)
        r = [a.b]
        s1v = s1
        s2v = s2
        if isinstance(s1, TV):
            r.append(s1.b)
            s1v = s1.ap
        if isinstance(s2, TV):
            r.append(s2.b)
            s2v = s2.ap
        if op1 is None:
            self.S.op(eng, lambda: e.tensor_scalar(out=out.ap, in0=a.ap, scalar1=s1v, scalar2=None, op0=op0), r, [out.b])
        else:
            self.S.op(eng, lambda: e.tensor_scalar(out=out.ap, in0=a.ap, scalar1=s1v, scalar2=s2v, op0=op0, op1=op1), r, [out.b])

    def stt(self, out, a, s, b, op0, op1, eng=DVE):
        e = self._ve(eng)
        r = [a.b, b.b]
        sv = s
        if isinstance(s, TV):
            r.append(s.b)
            sv = s.ap
        self.S.op(eng, lambda: e.scalar_tensor_tensor(out=out.ap, in0=a.ap, scalar=sv, in1=b.ap, op0=op0, op1=op1), r, [out.b])

    def cp(self, out, in_, eng=DVE):
        if eng == ACT:
            nc = self.nc
            self.S.op(ACT, lambda: nc.scalar.copy(out=out.ap, in_=in_.ap), [in_.b], [out.b])
        else:
            e = self._ve(eng)
            self.S.op(eng, lambda: e.tensor_copy(out=out.ap, in_=in_.ap), [in_.b], [out.b])

    def recip(self, out, in_):
        nc = self.nc
        self.S.op(DVE, lambda: nc.vector.reciprocal(out=out.ap, in_=in_.ap), [in_.b], [out.b])

    def reduce(self, out, in_, op, eng=DVE):
        e = self._ve(eng)
        self.S.op(eng, lambda: e.tensor_reduce(out=out.ap, in_=in_.ap, axis=mybir.AxisListType.X, op=op), [in_.b], [out.b])

    def memset(self, out, val, eng=DVE):
        e = self._ve(eng)
        self.S.op(eng, lambda: e.memset(out.ap, val), [], [out.b])

    def dma(self, out, in_, eng=SP):
        nc = self.nc
        q = nc.sync if eng == SP else nc.gpsimd
        self.S.dma(eng, lambda: q.dma_start(out=out.ap, in_=in_.ap), [in_.b], [out.b])


def rmsnorm_rstd(k, ss, rstd, n_feat, epst):
    k.act(rstd, ss, AF.Sqrt, bias=epst, scale=1.0 / n_feat)
    k.recip(rstd, rstd)


def norm_block(k, h_rows, ht, junk, ss, rstd, nb, wbc, epst, ident_b, nT, ps_pair, t, tr_dt=BF16, copy_engs=(ACT, DVE)):
    k.dma(ht, h_rows)
    k.act(junk, ht, AF.Square, accum=ss)
    rmsnorm_rstd(k, ss, rstd, D, epst)
    k.stt(nb, ht, rstd, wbc, ALU.mult, ALU.mult)
    for half in range(2):
        pb = ps_pair[half]
        pv = pb.v(lambda a: a.bitcast(BF16))
        for c in range(8):
            k.tr(pv[:, c * 128:(c + 1) * 128], nb[:, (half * 8 + c) * 128:(half * 8 + c + 1) * 128], ident_b)
        k.cp(nT[:, half * 8:(half + 1) * 8, t * 128:(t + 1) * 128],
             pv.re("p (c t) -> p c t", t=128), eng=copy_engs[half])


NA = 1664
NS = 1296
NCV = 1536


def build_mixer(S, stop=None):
    CK = int(os.environ.get('MK_CK', '0'))
    NB = S // 512
    NCH = S // 128
    nc = bass.Bass("TRN2", target_bir_lowering=False)
    with ExitStack() as st:
        k = KB(nc, st)
        X = lambda name, shape, dt=F32: k.dram(name, shape, dt, kind="ExternalInput")
        h = X("h", [S, D])
        pos = X("pos", [1, S], I32)
        anw = X("anw", [1, D])
        WA = X("WA", [128, 16 * NA])
        kvnw = X("kvnw", [128, 4])
        wuk = X("wuk", [128, 4 * 512])
        wuv = X("wuv", [128, 4 * 512])
        ropec = X("ropec", [128, 2])
        WS = X("WS", [128, 16 * NS])
        WC = X("WC", [128, 16 * NCV])
        convw = X("convw", [128, 6 * 5])
        convb = X("convb", [128, 6])
        alog = X("alog", [1, 16])
        dtb = X("dtb", [1, 16])
        dsk = X("dsk", [1, 8])
        snw = X("snw", [1, 512])
        scw = X("scw", [128, 4 * 3])
        ident_in = X("ident", [128, 128])
        masks_in = X("masks", [128, 4 * 128])
        yT = k.dram("yT", [1536, S], BF16, kind="ExternalOutput")
        nT_s = k.dram("nT_s", [128, 16, S], BF16)
        qn_s = k.dram("qn_s", [4, 128, S], BF16)
        qr_s = k.dram("qr_s", [4, 64, S], BF16)
        kn_s = k.dram("kn_s", [4, 128, S], BF16)
        kr_s = k.dram("kr_s", [64, S], BF16)
        v_s = k.dram("v_s", [S, 512], BF16)
        xbc_s = k.dram("xbc_s", [6, 128, S], F32)
        zs_s = k.dram("zs_s", [S, 512], F32)
        scb_s = k.dram("scb_s", [4, 128, S], F32)
        sccx_s = k.dram("sccx_s", [4, 128, S], F32)
        yt_s = k.dram("yt_s", [S, 512], F32)

        P = k.ps
        ident_f = k.alloc(128, F32, "ident_f")
        ident_b = k.alloc(128, BF16, "ident_b")
        masks = k.alloc(512, F32, "masks")
        ones_f = k.alloc(128, F32, "ones_f")
        ones_b = k.alloc(128, BF16, "ones_b")
        epst = k.alloc(1, F32, "eps")
        k.dma(ident_f, ident_in)
        k.dma(ident_b, ident_in, eng=POOL)
        k.dma(masks, masks_in)
        k.memset(ones_f, 1.0)
        k.memset(ones_b, 1.0)
        k.memset(epst, EPS)
        MU, ML, MSU, MSL = [masks[:, i * 128:(i + 1) * 128] for i in range(4)]

        k.mark()
        wa = k.alloc(16 * NA, BF16, "wa")
        wa3 = wa.re("p (c n) -> p c n", n=NA)
        for c4 in range(4):
            k.dma(wa[:, c4 * 4 * NA:(c4 + 1) * 4 * NA], WA[:, c4 * 4 * NA:(c4 + 1) * 4 * NA], eng=POOL)
        wukt = k.alloc(2048, BF16, "wuk")
        wuvt = k.alloc(2048, BF16, "wuv")
        k.dma(wukt, wuk, eng=POOL)
        k.dma(wuvt, wuv, eng=POOL)
        wuk3 = wukt.re("p (r n) -> p r n", n=512)
        wuv3 = wuvt.re("p (r n) -> p r n", n=512)
        kvnwt = k.alloc(4, F32, "kvnw")
        k.dma(kvnwt, kvnw)
        ropet = k.alloc(2, F32, "ropec")
        k.dma(ropet, ropec)
        wbc = k.alloc(D, F32, "anw_bc")
        k.dma(wbc, anw.v(lambda a: a.partition_broadcast(128)))
        hts = [k.alloc(D, F32, "ht%d" % i) for i in range(2)]
        junk = k.alloc(D, BF16, "junk")
        nbs = [k.alloc(D, BF16, "nb%d" % i) for i in range(2)]
        sss = [k.alloc(1, F32, "ss%d" % i) for i in range(2)]
        rstds = [k.alloc(1, F32, "rstd%d" % i) for i in range(2)]
        nTs = [k.alloc(16 * 512, BF16, "nT%d" % i) for i in range(2)]
        posi = k.alloc(512, I32, "posi")
        posf = k.alloc(512, F32, "posf")
        ang = k.alloc(512, F32, "ang")
        kq = k.alloc(512, F32, "kq")
        kqi = k.alloc(512, I32, "kqi")
        cos2 = k.alloc(512, F32, "cos2")
        sin2 = k.alloc(512, F32, "sin2")
        ckv = k.alloc(4 * 512, F32, "ckv").re("p (r t) -> p r t", t=512)
        sq = k.alloc(4 * 512, F32, "sq").re("p (r t) -> p r t", t=512)
        rs_bc = k.alloc(512, F32, "rs_bc")
        ckvn = k.alloc(4 * 512, BF16, "ckvn").re("p (r t) -> p r t", t=512)
        t1 = k.alloc(512, F32, "t1")
        t2 = k.alloc(512, F32, "t2")
        stg = [k.alloc(512, BF16, "stg%d" % i) for i in range(4)]
        stg_i = [0]

        def stage_out(psrc, dst, rows=128, eng=ACT, src_is_tv=True):
            s_ = stg[stg_i[0] % 4]
            stg_i[0] += 1
            k.cp(s_[:rows], psrc, eng=eng)
            k.dma(dst, s_[:rows], eng=POOL)
            return s_

        if CK == 1:
            k.S.emit(st)
            return nc
        for blk in range(NB):
            b0 = blk * 512
            nT = nTs[blk % 2]
            nT3 = nT.re("p (c t) -> p c t", t=512)
            for t in range(4):
                i2 = (blk * 4 + t) % 2
                norm_block(k, h[b0 + t * 128:b0 + (t + 1) * 128, :], hts[i2], junk, sss[i2], rstds[i2], nbs[i2],
                           wbc, epst, ident_b, nT3, (P[0], P[1]), t)
            if CK == 2:
                k.S.emit(st)
                return nc
            k.dma(nT_s[:, :, b0:b0 + 512], nT3, eng=POOL)
            k.dma(posi, pos[:, b0:b0 + 512].v(lambda a: a.partition_broadcast(128)))
            k.cp(posf, posi)
            k.ts(ang, posf, ropet[:, 0:1], ALU.mult)
            for (dst, shift) in ((sin2, 0.0), (cos2, float(np.pi / 2))):
                k.ts(kq, ang, shift, ALU.add, 1.0 / TWO_PI, ALU.mult)
                k.cp(kqi, kq)
                k.cp(kq, kqi)
                k.stt(t1, kq, -TWO_PI, ang, ALU.mult, ALU.add)
                if shift != 0.0:
                    k.ts(t1, t1, shift, ALU.add)
                k.act(dst, t1, AF.Sin)
            k.ts(sin2, sin2, ropet[:, 1:2], ALU.mult)
            pi = [2]

            def nextp():
                p = P[2 + (pi[0] % 6)]
                pi[0] += 1
                return p

            def proj(cols, M):
                p = nextp()
                for c in range(16):
                    k.mm(p[:M], wa3[:, c, cols:cols + M], nT3[:, c, :], start=(c == 0), stop=(c == 15))
                return p

            if CK == 3:
                k.S.emit(st)
                return nc
            for m in range(4):
                p = proj(m * 128, 128)
                stage_out(p, qn_s[m, :, b0:b0 + 512])
            if CK == 4:
                k.S.emit(st)
                return nc
            for (ca, cb, M, dsts) in ((512, 768, 128, (qr_s[0], qr_s[1])), (640, 896, 128, (qr_s[2], qr_s[3])),
                                      (1536, 1600, 64, (kr_s,))):
                pa = proj(ca, M)
                pb = proj(cb, M)
                k.tt(t1[:M], pa[:M], cos2[:M], ALU.mult)
                k.tt(t2[:M], pb[:M], sin2[:M], ALU.mult)
                s_ = stg[stg_i[0] % 4]
                stg_i[0] += 1
                k.tt(s_[:M], t1[:M], t2[:M], ALU.add)
                for di, dd in enumerate(dsts):
                    k.dma(dd[:, b0:b0 + 512], s_[di * 64:(di + 1) * 64], eng=POOL)
            if CK == 5:
                k.S.emit(st)
                return nc
            for r in range(4):
                p = proj(1024 + r * 128, 128)
                k.cp(ckv[:, r, :], p, eng=ACT)
                k.act(sq[:, r, :], p, AF.Square)
            p = nextp()
            for r in range(4):
                k.mm(p, ones_f, sq[:, r, :], start=(r == 0), stop=(r == 3))
            k.act(rs_bc, p, AF.Sqrt, bias=epst, scale=1.0 / 512)
            k.recip(rs_bc, rs_bc)
            for r in range(4):
                k.stt(ckvn[:, r, :], ckv[:, r, :], kvnwt[:, r:r + 1], rs_bc, ALU.mult, ALU.mult)
            if CK == 6:
                k.S.emit(st)
                return nc
            for m in range(4):
                p = nextp()
                for r in range(4):
                    k.mm(p, wuk3[:, r, m * 128:(m + 1) * 128], ckvn[:, r, :], start=(r == 0), stop=(r == 3))
                stage_out(p, kn_s[m, :, b0:b0 + 512])
            for t in range(4):
                p = nextp()
                for r in range(4):
                    k.mm(p, ckvn[:, r, t * 128:(t + 1) * 128], wuv3[:, r, :], start=(r == 0), stop=(r == 3))
                stage_out(p, v_s[b0 + t * 128:b0 + (t + 1) * 128, :], eng=DVE)
        k.release()
        if stop == "A":
            k.S.emit(st)
            return nc

        k.mark()
        scale = 192.0 ** -0.5
        kr = k.alloc(S, BF16, "kr")
        k.dma(kr[:64], kr_s)
        NQ = S // 512
        kns = [k.alloc(S, BF16, "kn%d" % i) for i in range(2)]
        vts = [k.alloc(S, BF16, "v%d" % i) for i in range(2)]
        qns = [k.alloc(S, BF16, "qn%d" % i) for i in range(2)]
        qrs = [k.alloc(S, BF16, "qr%d" % i) for i in range(2)]
        pTs = [k.alloc(512, BF16, "pT%d" % i) for i in range(3)]
        rsum = k.alloc(512, F32, "rsum")
        ostg = [k.alloc(512, BF16, "ostg%d" % i) for i in range(2)]
        for m in range(4):
            kn, vt, qn, qr = kns[m % 2], vts[m % 2], qns[m % 2], qrs[m % 2]
            k.dma(kn, kn_s[m])
            k.dma(qn, qn_s[m])
            k.dma(qr[:64], qr_s[m])
            v3 = vt.re("p (t c) -> p t c", c=128)
            for tq in range(0, NCH, 16):
                te = min(tq + 16, NCH)
                k.dma(v3[:, tq:te, :], v_s.v(lambda a: a.rearrange("(t p) c -> p t c", p=128)[:, tq:te, m * 128:(m + 1) * 128]))
            for qc in range(NQ):
                q0 = qc * 512
                po = P[3 + (qc % 2)]
                psm = P[5 + (qc % 2)]

                def qk(kt):
                    pl = P[kt % 3]
                    k.mm(pl, kn[:, kt * 128:(kt + 1) * 128], qn[:, q0:q0 + 512], start=True, stop=False)
                    k.mm(pl, kr[:64, kt * 128:(kt + 1) * 128], qr[:64, q0:q0 + 512], start=False, stop=True)

                qk(0)
                for kt in range(NCH):
                    if kt + 1 < NCH:
                        qk(kt + 1)
                    pT = pTs[kt % 3]
                    k.act(pT, P[kt % 3], AF.Exp, scale=scale)
                    k.mm(po, v3[:, kt, :], pT, start=(kt == 0), stop=(kt == NCH - 1))
                    k.mm(psm, ones_b, pT, start=(kt == 0), stop=(kt == NCH - 1))
                k.recip(rsum, psm)
                os_ = ostg[qc % 2]
                k.tt(os_, po, rsum, ALU.mult)
                k.dma(yT[m * 128:(m + 1) * 128, q0:q0 + 512], os_, eng=POOL)
        k.release()
        if stop == "B":
            k.S.emit(st)
            return nc

        dt_all = k.alloc(NCH * 16, F32, "dt_all").re("p (g n) -> p g n", n=16)
        a_all = k.alloc(NCH * 16, F32, "a_all").re("p (g n) -> p g n", n=16)
        aneg = k.alloc(16, F32, "aneg")
        dtb_bc = k.alloc(16, F32, "dtb_bc")
        d_bc = k.alloc(8, F32, "d_bc")
        snw_bc = k.alloc(512, F32, "snw_bc")
        cwt = k.alloc(30, F32, "convw").re("p (c k) -> p c k", k=5)
        cbt = k.alloc(6, F32, "convb")
        scwt = k.alloc(12, F32, "scw").re("p (c k) -> p c k", k=3)
        k.dma(aneg, alog.v(lambda a: a.partition_broadcast(128)))
        k.act(aneg, aneg, AF.Exp)
        k.ts(aneg, aneg, -1.0, ALU.mult)
        k.dma(dtb_bc, dtb.v(lambda a: a.partition_broadcast(128)))
        k.dma(d_bc, dsk.v(lambda a: a.partition_broadcast(128)))
        k.dma(snw_bc, snw.v(lambda a: a.partition_broadcast(128)))
        k.dma(cwt, convw.re("p (c k) -> p c k", k=5))
        k.dma(cbt, convb)
        k.dma(scwt, scw.re("p (c k) -> p c k", k=3))
        onet = k.alloc(1, F32, "one")
        k.memset(onet, 1.0)

        k.mark()
        ws = k.alloc(16 * NS, BF16, "ws")
        ws3 = ws.re("p (c n) -> p c n", n=NS)
        wc = k.alloc(16 * NCV, BF16, "wc")
        wc3 = wc.re("p (c n) -> p c n", n=NCV)
        for c4 in range(4):
            k.dma(ws[:, c4 * 4 * NS:(c4 + 1) * 4 * NS], WS[:, c4 * 4 * NS:(c4 + 1) * 4 * NS], eng=POOL)
        for c4 in range(4):
            k.dma(wc[:, c4 * 4 * NCV:(c4 + 1) * 4 * NCV], WC[:, c4 * 4 * NCV:(c4 + 1) * 4 * NCV], eng=POOL)
        nTs = [k.alloc(16 * 512, BF16, "nTc%d" % i) for i in range(2)]
        fst = [k.alloc(512, F32, "fst%d" % i) for i in range(4)]
        fi = [0]
        ctmp = k.alloc(512, F32, "ctmp")
        dtr = k.alloc(16, F32, "dtr")

        def fstage(psrc, dst, func=None, eng=ACT):
            s_ = fst[fi[0] % 4]
            fi[0] += 1
            if func is None:
                k.cp(s_, psrc, eng=eng)
            else:
                k.act(s_, psrc, func)
            k.dma(dst, s_, eng=POOL)

        for blk in range(NB):
            b0 = blk * 512
            nT3 = nTs[blk % 2].re("p (c t) -> p c t", t=512)
            k.dma(nT3, nT_s[:, :, b0:b0 + 512])
            pi = [0]

            def nextp():
                p = P[pi[0] % 8]
                pi[0] += 1
                return p

            for t in range(4):
                g = blk * 4 + t
                p = nextp()
                for c in range(16):
                    k.mm(p, nT3[:, c, t * 128:(t + 1) * 128], ws3[:, c, 0:512], start=(c == 0), stop=(c == 15))
                fstage(p, zs_s[b0 + t * 128:b0 + (t + 1) * 128, :], func=AF.Silu)
                p = nextp()
                for c in range(16):
                    k.mm(p[:, 0:16], nT3[:, c, t * 128:(t + 1) * 128], ws3[:, c, 1280:1296], start=(c == 0), stop=(c == 15))
                k.tt(dtr, p[:, 0:16], dtb_bc, ALU.add)
                k.act(dtr, dtr, AF.Exp)
                k.act(dt_all[:, g, :], dtr, AF.Ln, bias=onet, scale=1.0)
                k.tt(a_all[:, g, :], dt_all[:, g, :], aneg, ALU.mult)
            for c6 in range(6):
                p = nextp()
                for c in range(16):
                    k.mm(p, ws3[:, c, 512 + c6 * 128:512 + (c6 + 1) * 128], nT3[:, c, :], start=(c == 0), stop=(c == 15))
                fstage(p, xbc_s[c6, :, b0:b0 + 512], eng=(ACT if c6 % 2 == 0 else DVE))
            for c4 in range(4):
                p = nextp()
                for c in range(16):
                    k.mm(p, wc3[:, c, c4 * 128:(c4 + 1) * 128], nT3[:, c, :], start=(c == 0), stop=(c == 15))
                fstage(p, scb_s[c4, :, b0:b0 + 512])
                p1 = nextp()
                for c in range(16):
                    k.mm(p1, wc3[:, c, 512 + c4 * 128:512 + (c4 + 1) * 128], nT3[:, c, :], start=(c == 0), stop=(c == 15))
                p2 = nextp()
                for c in range(16):
                    k.mm(p2, wc3[:, c, 1024 + c4 * 128:1024 + (c4 + 1) * 128], nT3[:, c, :], start=(c == 0), stop=(c == 15))
                k.cp(ctmp, p1, eng=ACT)
                s_ = fst[fi[0] % 4]
                fi[0] += 1
                k.tt(s_, ctmp, p2, ALU.mult)
                k.dma(sccx_s[c4, :, b0:b0 + 512], s_, eng=POOL)
        k.release()
        if stop == "C1":
            k.S.emit(st)
            return nc

        k.mark()
        xws = [k.alloc(6 * 516, F32, "xw%d" % i).re("p (c t) -> p c t", t=516) for i in range(2)]
        xc = k.alloc(4 * 512, F32, "xc").re("p (c t) -> p c t", t=512)
        cacc = k.alloc(512, F32, "cacc")
        BTb = k.alloc(512, BF16, "BTb")
        CTb = k.alloc(512, BF16, "CTb")
        sbw = k.alloc(4 * 512, F32, "sbw").re("p (c t) -> p c t", t=512)
        scxw = k.alloc(4 * 514, F32, "scxw").re("p (c t) -> p c t", t=514)
        cvo = [k.alloc(512, BF16, "cvo%d" % i) for i in range(2)]
        Hs = k.alloc(512, F32, "H")
        Hb = k.alloc(512, BF16, "Hb")
        ecs = k.alloc(8, F32, "ecs")
        dch = k.alloc(8, F32, "dch")
        tmc = k.alloc(8, F32, "tmc")
        dst_ = k.alloc(8, F32, "dst")
        xdt = k.alloc(512, BF16, "xdt")
        xdtd = k.alloc(512, BF16, "xdtd")
        btok = k.alloc(128, BF16, "btok")
        dx = k.alloc(512, F32, "dx")
        Bm = k.alloc(1024, F32, "Bm")
        decT = k.alloc(1024, F32, "decT")
        cbm = k.alloc(128, F32, "cbm")
        MT = k.alloc(1024, BF16, "MT")
        y1 = k.alloc(512, F32, "y1")
        ys = [k.alloc(512, F32, "y%d" % i) for i in range(2)]
        ytl = [k.alloc(512, F32, "ytl%d" % i) for i in range(2)]
        zsl = [k.alloc(512, F32, "zsl%d" % i) for i in range(2)]
        ssn = k.alloc(1, F32, "ssn")
        rsn = k.alloc(1, F32, "rsn")
        yn = k.alloc(512, F32, "yn")
        yjunk = k.alloc(512, F32, "yjunk")
        yob = [k.alloc(512, BF16, "yob%d" % i) for i in range(2)]
        P1b = P[1].v(lambda a: a.bitcast(BF16))

        for d in range(2):
            Mcum, Mseg, Mstat, Mtri = (MU, MU, MSL, MU) if d == 0 else (ML, ML, MSU, ML)
            k.memset(Hs, 0.0)
            k.memset(Hb, 0.0)
            blks = list(range(NB)) if d == 0 else list(range(NB - 1, -1, -1))
            for bi, blk in enumerate(blks):
                b0 = blk * 512
                xw = xws[bi % 2]
                lo = max(b0 - 2, 0)
                hi = min(b0 + 514, S)
                if b0 - 2 < 0:
                    k.memset(xw[:, :, 0:2], 0.0)
                if b0 + 514 > S:
                    k.memset(xw[:, :, 514:516], 0.0)
                k.dma(xw[:, :, lo - (b0 - 2):hi - (b0 - 2)], xbc_s.v(lambda a: a.rearrange("c p t -> p c t")[:, :, lo:hi]))
                for c6 in range(6):
                    k.ts(cacc, xw[:, c6, 0:512], cwt[:, c6, 0:1], ALU.mult)
                    for kk in range(1, 5):
                        k.stt(cacc, xw[:, c6, kk:kk + 512], cwt[:, c6, kk:kk + 1], cacc, ALU.mult, ALU.add)
                    o = xc[:, c6, :] if c6 < 4 else (BTb if c6 == 4 else CTb)
                    k.act(o, cacc, AF.Silu, bias=cbt[:, c6:c6 + 1])
                if d == 0:
                    lo1 = max(b0 - 1, 0)
                    hi1 = min(b0 + 513, S)
                    if b0 - 1 < 0:
                        k.memset(scxw[:, :, 0:1], 0.0)
                    if b0 + 513 > S:
                        k.memset(scxw[:, :, 513:514], 0.0)
                    k.dma(scxw[:, :, lo1 - (b0 - 1):hi1 - (b0 - 1)], sccx_s.v(lambda a: a.rearrange("c p t -> p c t")[:, :, lo1:hi1]))
                    k.dma(sbw, scb_s.v(lambda a: a.rearrange("c p t -> p c t")[:, :, b0:b0 + 512]))
                    for c4 in range(4):
                        k.ts(cacc, scxw[:, c4, 0:512], scwt[:, c4, 0:1], ALU.mult)
                        for kk in range(1, 3):
                            k.stt(cacc, scxw[:, c4, kk:kk + 512], scwt[:, c4, kk:kk + 1], cacc, ALU.mult, ALU.add)
                        o = cvo[c4 % 2]
                        k.tt(o, cacc, sbw[:, c4, :], ALU.mult)
                        k.dma(yT[1024 + c4 * 128:1024 + (c4 + 1) * 128, b0:b0 + 512], o, eng=POOL)
                chunks = list(range(4)) if d == 0 else [3, 2, 1, 0]
                for ci, cc in enumerate(chunks):
                    g = blk * 4 + cc
                    c0 = cc * 128
                    r0 = g * 128
                    a_d = a_all[:, g, d * 8:(d + 1) * 8]
                    dt_d = dt_all[:, g, d * 8:(d + 1) * 8]
                    yv = ys[g % 2]
                    if d == 1:
                        k.dma(ytl[g % 2], yt_s[r0:r0 + 128, :])
                        k.dma(zsl[g % 2], zs_s[r0:r0 + 128, :])
                    for c4 in range(4):
                        k.tr(P[0][:, c4 * 128:(c4 + 1) * 128], xc[:, c4, c0:c0 + 128], ident_f)
                    k.tr(P1b[:, 0:128], BTb[:, c0:c0 + 128], ident_b)
                    k.mm(P[1][:, 256:264], Mcum, a_d)
                    k.mm(P[1][:, 264:272], ones_f, a_d)
                    k.act(ecs, P[1][:, 256:264], AF.Exp)
                    k.act(dch, P[1][:, 264:272], AF.Exp)
                    k.cp(tmc, P[1][:, 264:272])
                    k.tt(tmc, tmc, P[1][:, 256:264], ALU.subtract)
                    k.act(dst_, tmc, AF.Exp)
                    k.tt(xdt.re("p (h q) -> p h q", q=64), P[0].re("p (h q) -> p h q", q=64), dt_d.bc(2, [128, 8, 64]), ALU.mult)
                    k.tt(xdtd.re("p (h q) -> p h q", q=64), xdt.re("p (h q) -> p h q", q=64), dst_.bc(2, [128, 8, 64]), ALU.mult)
                    k.cp(btok, P1b[:, 0:128], eng=ACT)
                    if d == 0:
                        k.tt(dx.re("p (h q) -> p h q", q=64), P[0].re("p (h q) -> p h q", q=64), d_bc.bc(2, [128, 8, 64]), ALU.mult)
                    k.tt(Bm.re("p (h l) -> p h l", l=128), Mseg.bc(1, [128, 8, 128]), a_d.bc(2, [128, 8, 128]), ALU.mult)
                    k.mm(P[2], Mstat, Bm[:, 0:512])
                    k.mm(P[3], Mstat, Bm[:, 512:1024])
                    k.act(decT[:, 0:512], P[2], AF.Exp)
                    k.act(decT[:, 512:1024], P[3], AF.Exp)
                    k.mm(P[4][:, 0:128], BTb[:, c0:c0 + 128], CTb[:, c0:c0 + 128])
                    k.tt(cbm, P[4][:, 0:128], Mtri, ALU.mult)
                    k.tt(MT.re("p (h l) -> p h l", l=128), decT.re("p (h l) -> p h l", l=128), cbm.bc(1, [128, 8, 128]), ALU.mult)
                    for hh in range(8):
                        k.mm(P[5][:, hh * 64:(hh + 1) * 64], MT[:, hh * 128:(hh + 1) * 128], xdt[:, hh * 64:(hh + 1) * 64])
                    k.mm(P[6], CTb[:, c0:c0 + 128], Hb)
                    k.tt(y1.re("p (h q) -> p h q", q=64), P[6].re("p (h q) -> p h q", q=64), ecs.bc(2, [128, 8, 64]), ALU.mult)
                    k.tt(yv, y1, P[5], ALU.add)
                    if d == 0:
                        k.tt(yv, yv, dx, ALU.add)
                        k.dma(yt_s[r0:r0 + 128, :], yv, eng=POOL)
                    k.mm(P[7], btok, xdtd)
                    k.tt(Hs.re("p (h q) -> p h q", q=64), Hs.re("p (h q) -> p h q", q=64), dch.bc(2, [128, 8, 64]), ALU.mult)
                    k.tt(Hs, Hs, P[7], ALU.add)
                    k.cp(Hb, Hs, eng=ACT)
                    if d == 1:
                        k.tt(yv, yv, ytl[g % 2], ALU.add)
                        k.tt(yv, yv, zsl[g % 2], ALU.mult)
                        k.act(yjunk, yv, AF.Square, accum=ssn)
                        rmsnorm_rstd(k, ssn, rsn, 512, epst)
                        k.stt(yn, yv, rsn, snw_bc, ALU.mult, ALU.mult)
                        for c4 in range(4):
                            k.tr(P[0][:, c4 * 128:(c4 + 1) * 128], yn[:, c4 * 128:(c4 + 1) * 128], ident_f)
                        o = yob[g % 2]
                        k.cp(o, P[0], eng=ACT)
                        k.dma(yT[512:1024, r0:r0 + 128].v(lambda a: a.rearrange("(c p) t -> p c t", p=128)),
                              o.re("p (c t) -> p c t", t=128), eng=POOL)
            if d == 0:
                k.S.barrier()
        k.release()
        k.S.emit(st)
    return nc


def arr_w(W):
    K, n = W.shape
    return np.ascontiguousarray(W.reshape(K // 128, 128, n).transpose(1, 0, 2)).reshape(128, (K // 128) * n)


def mixer_consts():
    r = np.arange(128)[:, None]
    c = np.arange(128)[None, :]
    masks = np.concatenate([(r <= c), (r >= c), (r < c), (r > c)], axis=1).astype(np.float32)
    freq = (10000.0 ** (-np.arange(0, 64, 2, dtype=np.float32) / 64)).astype(np.float32)
    p = np.arange(128)
    ropec = np.stack([freq[p % 32], np.where((p % 64) < 32, -1.0, 1.0)], axis=1).astype(np.float32)
    return {"ident": np.eye(128, dtype=np.float32), "masks": masks, "ropec": ropec}


def mixer_weights(inp, l, j):
    W = inp["w_in"][l]
    hs = range(4 * j, 4 * j + 4)
    qn = [W[:, h * 192:h * 192 + 128] for h in hs]
    qr = [W[:, h * 192 + 128:h * 192 + 192] for h in hs]
    qrs = [np.concatenate([x[:, 32:64], x[:, 0:32]], axis=1) for x in qr]
    ckv = W[:, 3072:3584]
    krp = W[:, 3584:3648]
    krs = np.concatenate([krp[:, 32:64], krp[:, 0:32]], axis=1)
    WA = np.concatenate(qn + qr + qrs + [ckv, krp, krs], axis=1)
    z0, x0, d0 = 3648, 5696, 8768
    WS = np.concatenate([W[:, z0 + 512 * j:z0 + 512 * (j + 1)], W[:, x0 + 512 * j:x0 + 512 * (j + 1)],
                         W[:, x0 + 2048 + 128 * j:x0 + 2048 + 128 * (j + 1)],
                         W[:, x0 + 2560 + 128 * j:x0 + 2560 + 128 * (j + 1)],
                         W[:, d0 + 8 * j:d0 + 8 * (j + 1)], W[:, d0 + 32 + 8 * j:d0 + 32 + 8 * (j + 1)]], axis=1)
    s0 = 8832
    WC = np.concatenate([W[:, s0 + 2048 * i + 512 * j:s0 + 2048 * i + 512 * (j + 1)] for i in range(3)], axis=1)
    chans = np.concatenate([np.arange(512 * j, 512 * (j + 1)), 2048 + np.arange(128 * j, 128 * (j + 1)),
                            2560 + np.arange(128 * j, 128 * (j + 1))])
    cw = inp["ssm_conv_w"][l][:, chans]
    convw = np.ascontiguousarray(cw.T.reshape(6, 128, 5).transpose(1, 0, 2)).reshape(128, 30)
    convb = np.ascontiguousarray(inp["ssm_conv_b"][l][chans].reshape(6, 128).T)
    sw = inp["sconv_w"][l][:, 512 * j:512 * (j + 1)]
    scw = np.ascontiguousarray(sw.T.reshape(4, 128, 3).transpose(1, 0, 2)).reshape(128, 12)
    f = np.float32
    return {
        "anw": inp["attn_norm_w"][l][None, :].astype(f),
        "WA": arr_w(WA), "WS": arr_w(WS), "WC": arr_w(WC),
        "kvnw": np.ascontiguousarray(inp["kv_norm_w"][l].reshape(4, 128).T),
        "wuk": arr_w(inp["w_uk"][l][:, 512 * j:512 * (j + 1)]),
        "wuv": arr_w(inp["w_uv"][l][:, 512 * j:512 * (j + 1)]),
        "convw": convw, "convb": convb,
        "alog": np.concatenate([inp["ssm_A_log"][l][0, 8 * j:8 * j + 8], inp["ssm_A_log"][l][1, 8 * j:8 * j + 8]])[None, :],
        "dtb": np.concatenate([inp["ssm_dt_bias"][l][0, 8 * j:8 * j + 8], inp["ssm_dt_bias"][l][1, 8 * j:8 * j + 8]])[None, :],
        "dsk": inp["ssm_D"][l][None, 8 * j:8 * j + 8],
        "snw": inp["ssm_norm_w"][l][None, 512 * j:512 * (j + 1)],
        "scw": scw,
    }


NE = 32


def build_rest(T, stop=None):
    CK = int(os.environ.get('MK_CK2', '0'))
    NBLK = T // 512
    HT = min(1024, T)
    NH = T // HT
    TPH = HT // 128
    SPH = HT // 512
    nc = bass.Bass("TRN2", target_bir_lowering=False)
    with ExitStack() as st:
        k = KB(nc, st, arena_words=50176)
        X = lambda name, shape, dt=F32: k.dram(name, shape, dt, kind="ExternalInput")
        h = X("h", [T, D])
        yTa = X("yTa", [128, 48 * T], BF16)
        anw = X("anw", [1, D])
        fnw = X("fnw", [1, D])
        finw = X("finw", [1, D])
        WGs = X("WGs", [16, 128, 16 * 384])
        WBs = X("WBs", [16, 128, 48 * 128])
        WOa = X("WOa", [128, 16 * D])
        rw = X("rw", [128, 16 * 40])
        rb = X("rb", [1, 40])
        WGE = X("WGE", [NE, 4, 128, 16 * 128])
        WUE = X("WUE", [NE, 4, 128, 16 * 128])
        WDE = X("WDE", [NE, 4, 128, D])
        ident_in = X("ident", [128, 128])
        h_out = k.dram("h_out", [T, D], F32, kind="ExternalOutput")
        y_out = k.dram("y_out", [T, D], F32, kind="ExternalOutput")
        mT_s = k.dram("mT_s", [128, 16, T], BF16)
        hn_s = k.dram("hn_s", [T, D], F32)
        n2T_s = k.dram("n2T_s", [128, 16, T], BF16)
        P = k.ps
        pi = [0]

        def nextp():
            p = P[pi[0] % 8]
            pi[0] += 1
            return p

        ident_f = k.alloc(128, F32, "ident_f")
        ident_b = k.alloc(128, BF16, "ident_b")
        epst = k.alloc(1, F32, "eps")
        k.dma(ident_f, ident_in)
        k.dma(ident_b, ident_in, eng=POOL)
        k.memset(epst, EPS)
        comb_all = k.alloc((T // 128) * 32, F32, "comb").re("p (t e) -> p t e", e=32)
        yTa3 = yTa.re("p (k t) -> p k t", t=T)

        k.mark()
        wbc = k.alloc(D, F32, "anw_bc")
        k.dma(wbc, anw.v(lambda a: a.partition_broadcast(128)))
        hts = [k.alloc(D, F32, "ht%d" % i) for i in range(2)]
        junk = k.alloc(D, BF16, "junk")
        nbs = [k.alloc(D, BF16, "nb%d" % i) for i in range(2)]
        sss = [k.alloc(1, F32, "ss%d" % i) for i in range(2)]
        rstds = [k.alloc(1, F32, "rstd%d" % i) for i in range(2)]
        nT3 = k.alloc(16 * 512, BF16, "nT").re("p (c t) -> p c t", t=512)
        ytb = k.alloc(48 * 512, BF16, "ytb").re("p (k t) -> p k t", t=512)
        wgs = [k.alloc(16 * 384, BF16, "wg%d" % i).re("p (c n) -> p c n", n=384) for i in range(2)]
        wbs = [k.alloc(48 * 128, BF16, "wb%d" % i).re("p (k n) -> p k n", n=128) for i in range(2)]
        mTs = k.alloc(16 * 512, BF16, "mTs").re("p (m t) -> p m t", t=512)
        gts = [k.alloc(512, F32, "g%d" % i) for i in range(2)]
        macc = k.alloc(512, F32, "macc")
        tmp = k.alloc(512, F32, "tmp")
        for blk in range(NBLK):
            b0 = blk * 512
            for t in range(4):
                i2 = (blk * 4 + t) % 2
                norm_block(k, h[b0 + t * 128:b0 + (t + 1) * 128, :], hts[i2], junk, sss[i2], rstds[i2], nbs[i2],
                           wbc, epst, ident_b, nT3, (P[0], P[1]), t)
            for i in range(3):
                k.dma(ytb[:, i * 16:(i + 1) * 16, :], yTa3[:, i * 16:(i + 1) * 16, b0:b0 + 512])
            pi[0] = 2
            for m in range(16):
                wg = wgs[m % 2]
                wb = wbs[m % 2]
                k.dma(wg, WGs[m].re("p (c n) -> p c n", n=384), eng=POOL)
                k.dma(wb, WBs[m].re("p (k n) -> p k n", n=128), eng=POOL)
                for i in range(3):
                    pg = P[2 + (pi[0] % 6)]
                    pi[0] += 1
                    for c in range(16):
                        k.mm(pg, wg[:, c, i * 128:(i + 1) * 128], nT3[:, c, :], start=(c == 0), stop=(c == 15))
                    g = gts[i % 2]
                    k.act(g, pg, AF.Sigmoid)
                    py = P[2 + (pi[0] % 6)]
                    pi[0] += 1
                    for kk in range(16):
                        k.mm(py, wb[:, i * 16 + kk, :], ytb[:, i * 16 + kk, :], start=(kk == 0), stop=(kk == 15))
                    if i == 0:
                        k.tt(macc, g, py, ALU.mult)
                    elif i == 1:
                        k.tt(tmp, g, py, ALU.mult)
                        k.tt(macc, macc, tmp, ALU.add)
                    else:
                        k.tt(tmp, g, py, ALU.mult)
                        k.tt(mTs[:, m, :], macc, tmp, ALU.add)
            k.dma(mT_s[:, :, b0:b0 + 512], mTs, eng=POOL)
        k.release()
        if stop == "1a":
            k.S.emit(st)
            return nc

        k.mark()
        wo3 = k.alloc(16 * D, BF16, "wo").re("p (m n) -> p m n", n=D)
        for c4 in range(4):
            k.dma(wo3[:, c4 * 4:(c4 + 1) * 4, :], WOa.re("p (m n) -> p m n", n=D)[:, c4 * 4:(c4 + 1) * 4, :], eng=POOL)
        fbc = k.alloc(D, F32, "fnw_bc")
        k.dma(fbc, fnw.v(lambda a: a.partition_broadcast(128)))
        rwt = k.alloc(16 * 40, F32, "rw").re("p (c n) -> p c n", n=40)
        k.dma(rwt, rw.re("p (c n) -> p c n", n=40))
        rb_bc = k.alloc(40, F32, "rb_bc")
        k.dma(rb_bc, rb.v(lambda a: a.partition_broadcast(128)))
        mTb = [k.alloc(16 * 512, BF16, "mTb%d" % i).re("p (m t) -> p m t", t=512) for i in range(2)]
        hts = [k.alloc(D, F32, "htb%d" % i) for i in range(2)]
        junk = k.alloc(D, BF16, "junkb")
        n2 = k.alloc(D, F32, "n2")
        n2Tf = k.alloc(16 * 128, F32, "n2Tf").re("p (c t) -> p c t", t=128)
        n2Tb = [k.alloc(16 * 512, BF16, "n2Tb%d" % i).re("p (c t) -> p c t", t=512) for i in range(2)]
        ss = k.alloc(1, F32, "ssb")
        rstd = k.alloc(1, F32, "rstdb")
        lgt = k.alloc(40, F32, "lgt")
        sm = {nm: k.alloc(n_, F32, nm) for nm, n_ in (("gmax", 1), ("gsh", 8), ("gex", 8), ("gsum", 1), ("gw", 1),
                                                      ("oh", 8), ("sel", 32), ("eg", 4), ("m1", 1), ("mask1", 4),
                                                      ("eg2", 4), ("m2", 1), ("mask2", 4), ("dd", 1), ("w2", 1),
                                                      ("w1", 1), ("c1", 4), ("cg", 4))}
        if CK == 1:
            k.S.emit(st)
            return nc
        for blk in range(NBLK):
            b0 = blk * 512
            mb = mTb[blk % 2]
            k.dma(mb, mT_s[:, :, b0:b0 + 512])
            for t in range(4):
                gt = blk * 4 + t
                r0 = b0 + t * 128
                ht = hts[gt % 2]
                k.dma(ht, h[r0:r0 + 128, :])
                for n in range(4):
                    p = nextp()
                    for m in range(16):
                        k.mm(p, mb[:, m, t * 128:(t + 1) * 128], wo3[:, m, n * 512:(n + 1) * 512], start=(m == 0), stop=(m == 15))
                    k.tt(ht[:, n * 512:(n + 1) * 512], ht[:, n * 512:(n + 1) * 512], p, ALU.add)
                k.dma(hn_s[r0:r0 + 128, :], ht, eng=POOL)
                if CK == 2:
                    k.S.emit(st)
                    return nc
                k.act(junk, ht, AF.Square, accum=ss)
                rmsnorm_rstd(k, ss, rstd, D, epst)
                k.stt(n2, ht, rstd, fbc, ALU.mult, ALU.mult)
                nb_ = n2Tb[blk % 2]
                MV = int(os.environ.get('MK_V', '9'))
                for q in range(4 if MV >= 2 else 0):
                    bank = nextp()
                    for c in range(4):
                        k.tr(bank[:, c * 128:(c + 1) * 128], n2[:, (q * 4 + c) * 128:(q * 4 + c + 1) * 128], ident_f)
                    k.cp(n2Tf[:, q * 4:(q + 1) * 4, :], bank.re("p (c t) -> p c t", t=128), eng=ACT)
                    if MV >= 3:
                        k.cp(nb_[:, q * 4:(q + 1) * 4, t * 128:(t + 1) * 128], bank.re("p (c t) -> p c t", t=128), eng=DVE)
                if t == 3:
                    k.dma(n2T_s[:, :, b0:b0 + 512], nb_, eng=POOL)
                if CK == 3:
                    k.S.emit(st)
                    return nc
                p = nextp()
                for c in range(16):
                    k.mm(p[:, 0:40], n2Tf[:, c, :], rwt[:, c, :], start=(c == 0), stop=(c == 15))
                k.tt(lgt, p[:, 0:40], rb_bc, ALU.add)
                if CK == 4:
                    k.S.emit(st)
                    return nc
                lg = lgt[:, 0:8]
                le = lgt[:, 8:40]
                k.reduce(sm["gmax"], lg, ALU.max)
                k.ts(sm["gsh"], lg, sm["gmax"], ALU.subtract)
                k.act(sm["gex"], sm["gsh"], AF.Exp, accum=sm["gsum"])
                k.recip(sm["gw"], sm["gsum"])
                k.ts(sm["oh"], lg, sm["gmax"], ALU.is_equal)
                k.tt(sm["sel"].re("p (g e) -> p g e", e=4), le.re("p (g e) -> p g e", e=4), sm["oh"].bc(2, [128, 8, 4]), ALU.mult)
                k.reduce(sm["eg"], sm["sel"].re("p (g e) -> p e g", e=4), ALU.add)
                k.reduce(sm["m1"], sm["eg"], ALU.max)
                k.ts(sm["mask1"], sm["eg"], sm["m1"], ALU.is_equal)
                k.stt(sm["eg2"], sm["mask1"], -1e30, sm["eg"], ALU.mult, ALU.add)
                k.reduce(sm["m2"], sm["eg2"], ALU.max)
                k.ts(sm["mask2"], sm["eg2"], sm["m2"], ALU.is_equal)
                k.tt(sm["dd"], sm["m2"], sm["m1"], ALU.subtract)
                k.act(sm["w2"], sm["dd"], AF.Sigmoid)
                k.ts(sm["w1"], sm["w2"], -1.0, ALU.mult, 1.0, ALU.add)
                k.ts(sm["c1"], sm["mask1"], sm["w1"], ALU.mult)
                k.stt(sm["cg"], sm["mask2"], sm["w2"], sm["c1"], ALU.mult, ALU.add)
                k.ts(sm["cg"], sm["cg"], sm["gw"], ALU.mult)
                k.tt(comb_all[:, gt, :].re("p (g e) -> p g e", e=4), sm["oh"].bc(2, [128, 8, 4]), sm["cg"].bc(1, [128, 8, 4]), ALU.mult)
        k.release()
        if stop == "1b":
            k.S.emit(st)
            return nc

        k.mark()
        n2T = k.alloc(16 * HT, BF16, "n2T").re("p (c t) -> p c t", t=HT)
        accs = [k.alloc(D, F32, "acc%d" % i) for i in range(TPH)]
        gu_ring = [k.alloc(16 * 128, BF16, "gu%d" % i).re("p (c f) -> p c f", f=128) for i in range(12)]
        d_ring = [k.alloc(D, BF16, "dw%d" % i) for i in range(6)]
        actT = k.alloc(4 * HT, BF16, "actT").re("p (j t) -> p j t", t=HT)
        sgs = [k.alloc(512, F32, "sg%d" % i) for i in range(2)]
        fin_bc = k.alloc(D, F32, "finw_bc")
        k.dma(fin_bc, finw.v(lambda a: a.partition_broadcast(128)))
        junk2 = k.alloc(D, BF16, "junk2")
        ss2 = k.alloc(1, F32, "ss2")
        rs2 = k.alloc(1, F32, "rs2")
        gi = [0]
        di = [0]
        sgi = [0]
        for hf in range(NH):
            t0 = hf * HT
            k.dma(n2T, n2T_s[:, :, t0:t0 + HT])
            for t in range(TPH):
                k.dma(accs[t], hn_s[t0 + t * 128:t0 + (t + 1) * 128, :])
            for e in range(NE):
                for j in range(4):
                    wg_ = gu_ring[gi[0] % 12]
                    gi[0] += 1
                    wu_ = gu_ring[gi[0] % 12]
                    gi[0] += 1
                    k.dma(wg_, WGE[e, j].re("p (c f) -> p c f", f=128), eng=POOL)
                    k.dma(wu_, WUE[e, j].re("p (c f) -> p c f", f=128), eng=POOL)
                    for sub in range(SPH):
                        pg = nextp()
                        for c in range(16):
                            k.mm(pg, wg_[:, c, :], n2T[:, c, sub * 512:(sub + 1) * 512], start=(c == 0), stop=(c == 15))
                        pu = nextp()
                        for c in range(16):
                            k.mm(pu, wu_[:, c, :], n2T[:, c, sub * 512:(sub + 1) * 512], start=(c == 0), stop=(c == 15))
                        sg = sgs[sgi[0] % 2]
                        sgi[0] += 1
                        k.act(sg, pg, AF.Silu)
                        k.tt(actT[:, j, sub * 512:(sub + 1) * 512], sg, pu, ALU.mult)
                wd = []
                for j in range(4):
                    w_ = d_ring[di[0] % 6]
                    di[0] += 1
                    k.dma(w_, WDE[e, j], eng=POOL)
                    wd.append(w_)
                for t in range(TPH):
                    for n in range(4):
                        p = nextp()
                        for j in range(4):
                            k.mm(p, actT[:, j, t * 128:(t + 1) * 128], wd[j][:, n * 512:(n + 1) * 512], start=(j == 0), stop=(j == 3))
                        k.stt(accs[t][:, n * 512:(n + 1) * 512], p, comb_all[:, hf * TPH + t, e:e + 1],
                              accs[t][:, n * 512:(n + 1) * 512], ALU.mult, ALU.add)
            for t in range(TPH):
                r0 = t0 + t * 128
                k.dma(h_out[r0:r0 + 128, :], accs[t], eng=SP)
                k.act(junk2, accs[t], AF.Square, accum=ss2)
                rmsnorm_rstd(k, ss2, rs2, D, epst)
                k.stt(accs[t], accs[t], rs2, fin_bc, ALU.mult, ALU.mult)
                k.dma(y_out[r0:r0 + 128, :], accs[t], eng=SP)
        k.release()
        k.S.emit(st)
    return nc


def rest_weights(inp, l):
    f = np.float32
    W = inp["w_in"][l][:, 14976:21120]
    WGs = np.ascontiguousarray(W.reshape(16, 128, 3, 16, 128).transpose(3, 1, 0, 2, 4)).reshape(16, 128, 16 * 384)
    WB = inp["w_branch"][l]
    WBs = np.ascontiguousarray(WB.reshape(48, 128, 16, 128).transpose(2, 1, 0, 3)).reshape(16, 128, 48 * 128)
    rw = np.concatenate([inp["router_group_w"][l], inp["router_expert_w"][l]], axis=1)
    rb = np.concatenate([inp["router_group_b"][l], inp["router_expert_b"][l]])[None, :]
    eg = inp["expert_w_gate"][l]
    eu = inp["expert_w_up"][l]
    ed = inp["expert_w_down"][l]
    WGE = np.ascontiguousarray(eg.reshape(NE, 16, 128, 4, 128).transpose(0, 3, 2, 1, 4)).reshape(NE, 4, 128, 16 * 128)
    WUE = np.ascontiguousarray(eu.reshape(NE, 16, 128, 4, 128).transpose(0, 3, 2, 1, 4)).reshape(NE, 4, 128, 16 * 128)
    WDE = np.ascontiguousarray(ed.reshape(NE, 4, 128, D))
    return {
        "anw": inp["attn_norm_w"][l][None, :].astype(f), "fnw": inp["ffn_norm_w"][l][None, :].astype(f),
        "finw": inp["final_norm_w"][None, :].astype(f),
        "WGs": WGs, "WBs": WBs, "WOa": arr_w(inp["w_o"][l]), "rw": arr_w(rw), "rb": np.ascontiguousarray(rb),
        "WGE": WGE, "WUE": WUE, "WDE": WDE, "ident": np.eye(128, dtype=f),
    }


def arr_yT(yT_full, t0, T):
    blk = yT_full[:, t0:t0 + T]
    return np.ascontiguousarray(blk.reshape(48, 128, T).transpose(1, 0, 2)).reshape(128, 48 * T)


_NC_CACHE = {}


def kernel(**inputs):
    inp = {k_: np.asarray(v) for k_, v in inputs.items()}
    B, S, _ = inp["x"].shape
    L = inp["w_in"].shape[0]
    T = (B * S) // 8
    CPB = 8 // B
    if "mixer" not in _NC_CACHE:
        _NC_CACHE["mixer"] = build_mixer(S)
        _NC_CACHE["rest"] = build_rest(T)
    consts = mixer_consts()
    hcur = np.ascontiguousarray(inp["x"], dtype=np.float32)
    out = None
    for l in range(L):
        maps = []
        wj = [mixer_weights(inp, l, j) for j in range(4)]
        for core in range(8):
            b, j = core // 4, core % 4
            m = dict(consts)
            m.update(wj[j])
            m["h"] = hcur[b]
            m["pos"] = inp["positions"][b][None, :]
            maps.append({k_: np.ascontiguousarray(v) for k_, v in m.items()})
        res = run_bass_kernel_spmd(_NC_CACHE["mixer"], maps, core_ids=list(range(8)))
        del maps
        yT_b = []
        for b in range(B):
            parts = [np.asarray(res.results[b * 4 + j]["yT"]) for j in range(4)]
            yT_b.append(np.concatenate([p_[i * 512:(i + 1) * 512] for i in range(3) for p_ in parts], axis=0))
        rwts = rest_weights(inp, l)
        maps = []
        for core in range(8):
            b, q = core // CPB, core % CPB
            m = dict(rwts)
            m["h"] = np.ascontiguousarray(hcur[b, q * T:(q + 1) * T])
            m["yTa"] = arr_yT(yT_b[b], q * T, T)
            maps.append(m)
        res = run_bass_kernel_spmd(_NC_CACHE["rest"], maps, core_ids=list(range(8)))
        del maps, rwts
        hn = np.stack([np.concatenate([np.asarray(res.results[b * CPB + q]["h_out"]) for q in range(CPB)], axis=0) for b in range(B)])
        if l == L - 1:
            out = np.stack([np.concatenate([np.asarray(res.results[b * CPB + q]["y_out"]) for q in range(CPB)], axis=0) for b in range(B)])
        hcur = hn.astype(np.float32)
    return out.astype(np.float32)
```

```python
import os
import numpy as np
from contextlib import ExitStack
import concourse.bass as bass
import concourse.mybir as mybir
from concourse.bass_utils import run_bass_kernel_spmd

F32 = mybir.dt.float32
BF16 = mybir.dt.bfloat16
I32 = mybir.dt.int32
ALU = mybir.AluOpType
AF = mybir.ActivationFunctionType
PE, ACT, DVE, POOL, SP = "pe", "act", "dve", "pool", "sp"

D = 2048
EPS = 1e-6
IN_COLS = 21120
TWO_PI = float(2 * np.pi)


class Buf:
    __slots__ = ("name", "last_w", "readers", "excl")

    def __init__(self, name="", excl=False):
        self.name = name
        self.last_w = None
        self.readers = []
        self.excl = excl


class Op:
    __slots__ = ("eng", "fn", "reads", "writes", "is_dma", "deps", "needs_inc",
                 "inc_val", "dma_sem", "dma_val", "dma_prev")

    def __init__(self, eng, fn, reads, writes, is_dma):
        self.eng = eng
        self.fn = fn
        self.reads = reads
        self.writes = writes
        self.is_dma = is_dma
        self.deps = ()
        self.needs_inc = False
        self.inc_val = 0
        self.dma_sem = None
        self.dma_val = 0
        self.dma_prev = None


class Sched:
    N_DMA_SEMS = 24
    SAME_ENGINE_SYNC = (ACT, DVE, POOL)

    def __init__(self, nc):
        self.nc = nc
        self.ops = []
        self.engs = {PE: nc.tensor, ACT: nc.scalar, DVE: nc.vector, POOL: nc.gpsimd, SP: nc.sync}
        self.store_bufs = []

    def op(self, eng, fn, reads=(), writes=()):
        self.ops.append(Op(eng, fn, tuple(b for b in reads if b is not None),
                           tuple(b for b in writes if b is not None), False))

    def dma(self, eng, fn, reads=(), writes=()):
        r = tuple(b for b in reads if b is not None)
        self.store_bufs.extend(r)
        self.ops.append(Op(eng, fn, r, tuple(b for b in writes if b is not None), True))

    def barrier(self):
        self.store_bufs = []
        self.ops.append(Op(None, None, (), (), False))

    def emit(self, stack):
        nc = self.nc
        ops = self.ops
        last_on = {}
        for i, o in enumerate(ops):
            if o.eng is None:
                for e, li in last_on.items():
                    ops[li].needs_inc = True
                continue
            if not o.is_dma:
                last_on[o.eng] = i
            deps = set()
            for b in o.reads:
                if b.last_w is not None:
                    deps.add(b.last_w)
                if b.excl:
                    deps.update(r for r in b.readers if ops[r].eng != o.eng)
            for b in o.writes:
                if b.last_w is not None:
                    deps.add(b.last_w)
                deps.update(b.readers)
            for b in o.writes:
                b.last_w = i
                b.readers = []
            for b in o.reads:
                if b.last_w != i:
                    b.readers.append(i)
            deps.discard(i)
            o.deps = deps
        for i, o in enumerate(ops):
            if o.eng is None:
                continue
            nd = []
            for d in o.deps:
                p = ops[d]
                if p.is_dma:
                    nd.append(d)
                    continue
                if p.eng == o.eng and not o.is_dma and o.eng not in self.SAME_ENGINE_SYNC:
                    continue
                nd.append(d)
                p.needs_inc = True
            o.deps = nd
        esem = {e: stack.enter_context(nc.semaphore("s_" + e)) for e in self.engs}
        dsems = {e: [stack.enter_context(nc.semaphore("d_%s_%d" % (e, k))) for k in range(self.N_DMA_SEMS)]
                 for e in (SP, POOL)}
        cnt = {e: 0 for e in self.engs}
        dcount = {e: 0 for e in dsems}
        dlast = {e: [None] * self.N_DMA_SEMS for e in dsems}
        dval = {e: [0] * self.N_DMA_SEMS for e in dsems}
        for i, o in enumerate(ops):
            if o.eng is None:
                continue
            if o.is_dma:
                k = dcount[o.eng] % self.N_DMA_SEMS
                dcount[o.eng] += 1
                o.dma_sem = dsems[o.eng][k]
                o.dma_prev = dlast[o.eng][k]
                dval[o.eng][k] += 16
                o.dma_val = dval[o.eng][k]
                dlast[o.eng][k] = i
            elif o.needs_inc:
                cnt[o.eng] += 1
                o.inc_val = cnt[o.eng]
        waited = {e: {} for e in self.engs}
        cur_cnt = {e: 0 for e in self.engs}
        cur_d = {}
        for i, o in enumerate(ops):
            if o.eng is None:
                for f in self.engs:
                    ef = self.engs[f]
                    wf = waited[f]
                    for e in self.engs:
                        if e != f and cur_cnt[e] > wf.get(("e", e), 0):
                            ef.wait_ge(esem[e], cur_cnt[e])
                            wf[("e", e)] = cur_cnt[e]
                    for key, (sem, val) in cur_d.items():
                        if val > wf.get(key, 0):
                            ef.wait_ge(sem, val)
                            wf[key] = val
                continue
            eng = self.engs[o.eng]
            w = waited[o.eng]
            need = {}
            dl = list(o.deps)
            if o.is_dma and o.dma_prev is not None:
                dl.append(o.dma_prev)
            for d in dl:
                p = ops[d]
                if p.is_dma:
                    key = ("d", p.eng, id(p.dma_sem))
                    sem, val = p.dma_sem, p.dma_val
                else:
                    key = ("e", p.eng)
                    sem, val = esem[p.eng], p.inc_val
                if w.get(key, 0) >= val:
                    continue
                if key not in need or need[key][1] < val:
                    need[key] = (sem, val)
            for key, (sem, val) in need.items():
                eng.wait_ge(sem, val)
                w[key] = val
            ins = o.fn()
            if o.is_dma:
                ins.then_inc(o.dma_sem, 16)
                cur_d[("d", o.eng, id(o.dma_sem))] = (o.dma_sem, o.dma_val)
            elif o.needs_inc:
                ins.then_inc(esem[o.eng], 1)
                cur_cnt[o.eng] = o.inc_val
        sp = self.engs[SP]
        for e in dsems:
            for k in range(self.N_DMA_SEMS):
                if dval[e][k] > 0:
                    sp.wait_ge(dsems[e][k], dval[e][k])
        for e in self.engs:
            if e != SP and cnt[e] > 0:
                sp.wait_ge(esem[e], cnt[e])


class TV:
    __slots__ = ("ap", "b")

    def __init__(self, ap, b):
        self.ap = ap
        self.b = b

    def __getitem__(self, k):
        return TV(self.ap[k], self.b)

    def v(self, f):
        return TV(f(self.ap), self.b)

    def re(self, s, **kw):
        return TV(self.ap.rearrange(s, **kw), self.b)

    def bc(self, axis, shape):
        return TV(self.ap.unsqueeze(axis).to_broadcast(shape), self.b)


class KB:
    def __init__(self, nc, st, arena_words=49152):
        self.nc = nc
        self.st = st
        self.S = Sched(nc)
        self.arena = st.enter_context(nc.sbuf_tensor("arena", [128, arena_words], F32))
        self.words = arena_words
        self.off = 0
        self.marks = []
        self.ps = []
        for i in range(8):
            p = st.enter_context(nc.psum_tensor("ps%d" % i, [128, 512], F32))
            self.ps.append(TV(p[:, :], Buf("ps%d" % i, excl=True)))

    def alloc(self, n, dt=F32, name=""):
        w = n if dt in (F32, I32) else (n + 1) // 2
        assert self.off + w <= self.words, "arena overflow %s: %d + %d > %d" % (name, self.off, w, self.words)
        ap = self.arena[:, self.off:self.off + w]
        self.off += w
        if dt != F32:
            ap = ap.bitcast(dt)
        return TV(ap, Buf(name))

    def mark(self):
        self.marks.append(self.off)

    def release(self):
        self.off = self.marks.pop()
        self.S.barrier()

    def dram(self, name, shape, dt, kind="Internal"):
        return TV(self.nc.dram_tensor(name, list(shape), dt, kind=kind).ap(), None)

    def mm(self, out, lhsT, rhs, start=True, stop=True, extra_r=()):
        nc = self.nc
        self.S.op(PE, lambda: nc.tensor.matmul(out.ap, lhsT.ap, rhs.ap, start=start, stop=stop),
                  [lhsT.b, rhs.b] + [x.b for x in extra_r], [out.b])

    def tr(self, out, in_, ident):
        nc = self.nc
        self.S.op(PE, lambda: nc.tensor.transpose(out.ap, in_.ap, ident.ap), [in_.b, ident.b], [out.b])

    def act(self, out, in_, func, bias=None, scale=1.0, accum=None):
        nc = self.nc
        kw = {}
        r = [in_.b]
        w = [out.b]
        if bias is not None:
            kw["bias"] = bias.ap
            r.append(bias.b)
        if accum is not None:
            kw["accum_out"] = accum.ap
            w.append(accum.b)
        self.S.op(ACT, lambda: nc.scalar.activation(out=out.ap, in_=in_.ap, func=func, scale=scale, **kw), r, w)

    def _ve(self, eng):
        return self.nc.vector if eng == DVE else self.nc.gpsimd

    def tt(self, out, a, b, op, eng=DVE):
        e = self._ve(eng)
        self.S.op(eng, lambda: e.tensor_tensor(out=out.ap, in0=a.ap, in1=b.ap, op=op), [a.b, b.b], [out.b])

    def ts(self, out, a, s1, op0, s2=None, op1=None, eng=DVE):
        e = self._ve(eng)
        r = [a.b]
        s1v = s1
        s2v = s2
        if isinstance(s1, TV):
            r.append(s1.b)
            s1v = s1.ap
        if isinstance(s2, TV):
            r.append(s2.b)
            s2v = s2.ap
        if op1 is None:
            self.S.op(eng, lambda: e.tensor_scalar(out=out.ap, in0=a.ap, scalar1=s1v, scalar2=None, op0=op0), r, [out.b])
        else:
            self.S.op(eng, lambda: e.tensor_scalar(out=out.ap, in0=a.ap, scalar1=s1v, scalar2=s2v, op0=op0, op1=op1), r, [out.b])

    def stt(self, out, a, s, b, op0, op1, eng=DVE):
        e = self._ve(eng)
        r = [a.b, b.b]
        sv = s
        if isinstance(s, TV):
            r.append(s.b)
            sv = s.ap
        self.S.op(eng, lambda: e.scalar_tensor_tensor(out=out.ap, in0=a.ap, scalar=sv, in1=b.ap, op0=op0, op1=op1), r, [out.b])

    def cp(self, out, in_, eng=DVE):
        if eng == ACT:
            nc = self.nc
            self.S.op(ACT, lambda: nc.scalar.copy(out=out.ap, in_=in_.ap), [in_.b], [out.b])
        else:
            e = self._ve(eng)
            self.S.op(eng, lambda: e.tensor_copy(out=out.ap, in_=in_.ap), [in_.b], [out.b])

    def recip(self, out, in_):
        nc = self.nc
        self.S.op(DVE, lambda: nc.vector.reciprocal(out=out.ap, in_=in_.ap), [in_.b], [out.b])

    def reduce(self, out, in_, op, eng=DVE):
        e = self._ve(eng)
        self.S.op(eng, lambda: e.tensor_reduce(out=out.ap, in_=in_.ap, axis=mybir.AxisListType.X, op=op), [in_.b], [out.b])

    def memset(self, out, val, eng=DVE):
        e = self._ve(eng)
        self.S.op(eng, lambda: e.memset(out.ap, val), [], [out.b])

    def dma(self, out, in_, eng=SP):
        nc = self.nc
        q = nc.sync if eng == SP else nc.gpsimd
        self.S.dma(eng, lambda: q.dma_start(out=out.ap, in_=in_.ap), [in_.b], [out.b])


def rmsnorm_rstd(k, ss, rstd, n_feat, epst):
    k.act(rstd, ss, AF.Sqrt, bias=epst, scale=1.0 / n_feat)
    k.recip(rstd, rstd)


def norm_block(k, h_rows, ht, junk, ss, rstd, nb, wbc, epst, ident_b, nT, ps_pair, t, tr_dt=BF16, copy_engs=(ACT, DVE)):
    k.dma(ht, h_rows)
    k.act(junk, ht, AF.Square, accum=ss)
    rmsnorm_rstd(k, ss, rstd, D, epst)
    k.stt(nb, ht, rstd, wbc, ALU.mult, ALU.mult)
    for half in range(2):
        pb = ps_pair[half]
        pv = pb.v(lambda a: a.bitcast(BF16))
        for c in range(8):
            k.tr(pv[:, c * 128:(c + 1) * 128], nb[:, (half * 8 + c) * 128:(half * 8 + c + 1) * 128], ident_b)
        k.cp(nT[:, half * 8:(half + 1) * 8, t * 128:(t + 1) * 128],
             pv.re("p (c t) -> p c t", t=128), eng=copy_engs[half])


NA = 1664
NS = 1296
NCV = 1536


def build_mixer(S, stop=None):
    CK = int(os.environ.get('MK_CK', '0'))
    NB = S // 512
    NCH = S // 128
    nc = bass.Bass("TRN2", target_bir_lowering=False)
    with ExitStack() as st:
        k = KB(nc, st)
        X = lambda name, shape, dt=F32: k.dram(name, shape, dt, kind="ExternalInput")
        h = X("h", [S, D])
        pos = X("pos", [1, S], I32)
        anw = X("anw", [1, D])
        WA = X("WA", [128, 16 * NA])
        kvnw = X("kvnw", [128, 4])
        wuk = X("wuk", [128, 4 * 512])
        wuv = X("wuv", [128, 4 * 512])
        ropec = X("ropec", [128, 2])
        WS = X("WS", [128, 16 * NS])
        WC = X("WC", [128, 16 * NCV])
        convw = X("convw", [128, 6 * 5])
        convb = X("convb", [128, 6])
        alog = X("alog", [1, 16])
        dtb = X("dtb", [1, 16])
        dsk = X("dsk", [1, 8])
        snw = X("snw", [1, 512])
        scw = X("scw", [128, 4 * 3])
        ident_in = X("ident", [128, 128])
        masks_in = X("masks", [128, 4 * 128])
        yT = k.dram("yT", [1536, S], BF16, kind="ExternalOutput")
        nT_s = k.dram("nT_s", [128, 16, S], BF16)
        qn_s = k.dram("qn_s", [4, 128, S], BF16)
        qr_s = k.dram("qr_s", [4, 64, S], BF16)
        kn_s = k.dram("kn_s", [4, 128, S], BF16)
        kr_s = k.dram("kr_s", [64, S], BF16)
        v_s = k.dram("v_s", [S, 512], BF16)
        xbc_s = k.dram("xbc_s", [6, 128, S], F32)
        zs_s = k.dram("zs_s", [S, 512], F32)
        scb_s = k.dram("scb_s", [4, 128, S], F32)
        sccx_s = k.dram("sccx_s", [4, 128, S], F32)
        yt_s = k.dram("yt_s", [S, 512], F32)
        xcs_s = k.dram("xcs_s", [S // 512, 128, 4 * 512], F32)
        bt_s = k.dram("bt_s", [S // 512, 128, 512], BF16)
        ct_s = k.dram("ct_s", [S // 512, 128, 512], BF16)

        P = k.ps
        ident_f = k.alloc(128, F32, "ident_f")
        ident_b = k.alloc(128, BF16, "ident_b")
        masks = k.alloc(512, F32, "masks")
        ones_f = k.alloc(128, F32, "ones_f")
        ones_b = k.alloc(128, BF16, "ones_b")
        epst = k.alloc(1, F32, "eps")
        k.dma(ident_f, ident_in)
        k.dma(ident_b, ident_in, eng=POOL)
        k.dma(masks, masks_in)
        k.memset(ones_f, 1.0)
        k.memset(ones_b, 1.0)
        k.memset(epst, EPS)
        MU, ML, MSU, MSL = [masks[:, i * 128:(i + 1) * 128] for i in range(4)]

        k.mark()
        wa = k.alloc(16 * NA, BF16, "wa")
        wa3 = wa.re("p (c n) -> p c n", n=NA)
        for c4 in range(4):
            k.dma(wa[:, c4 * 4 * NA:(c4 + 1) * 4 * NA], WA[:, c4 * 4 * NA:(c4 + 1) * 4 * NA], eng=POOL)
        wukt = k.alloc(2048, BF16, "wuk")
        wuvt = k.alloc(2048, BF16, "wuv")
        k.dma(wukt, wuk, eng=POOL)
        k.dma(wuvt, wuv, eng=POOL)
        wuk3 = wukt.re("p (r n) -> p r n", n=512)
        wuv3 = wuvt.re("p (r n) -> p r n", n=512)
        kvnwt = k.alloc(4, F32, "kvnw")
        k.dma(kvnwt, kvnw)
        ropet = k.alloc(2, F32, "ropec")
        k.dma(ropet, ropec)
        wbc = k.alloc(D, F32, "anw_bc")
        k.dma(wbc, anw.v(lambda a: a.partition_broadcast(128)))
        hts = [k.alloc(D, F32, "ht%d" % i) for i in range(2)]
        junk = k.alloc(D, BF16, "junk")
        nbs = [k.alloc(D, BF16, "nb%d" % i) for i in range(2)]
        sss = [k.alloc(1, F32, "ss%d" % i) for i in range(2)]
        rstds = [k.alloc(1, F32, "rstd%d" % i) for i in range(2)]
        nTs = [k.alloc(16 * 512, BF16, "nT%d" % i) for i in range(2)]
        posi = k.alloc(512, I32, "posi")
        posf = k.alloc(512, F32, "posf")
        ang = k.alloc(512, F32, "ang")
        kq = k.alloc(512, F32, "kq")
        kqi = k.alloc(512, I32, "kqi")
        cos2 = k.alloc(512, F32, "cos2")
        sin2 = k.alloc(512, F32, "sin2")
        ckv = k.alloc(4 * 512, F32, "ckv").re("p (r t) -> p r t", t=512)
        sq = k.alloc(4 * 512, F32, "sq").re("p (r t) -> p r t", t=512)
        rs_bc = k.alloc(512, F32, "rs_bc")
        ckvn = k.alloc(4 * 512, BF16, "ckvn").re("p (r t) -> p r t", t=512)
        t1 = k.alloc(512, F32, "t1")
        t2 = k.alloc(512, F32, "t2")
        stg = [k.alloc(512, BF16, "stg%d" % i) for i in range(4)]
        stg_i = [0]

        def stage_out(psrc, dst, rows=128, eng=ACT, src_is_tv=True):
            s_ = stg[stg_i[0] % 4]
            stg_i[0] += 1
            k.cp(s_[:rows], psrc, eng=eng)
            k.dma(dst, s_[:rows], eng=POOL)
            return s_

        if CK == 1:
            k.S.emit(st)
            return nc
        for blk in range(NB):
            b0 = blk * 512
            nT = nTs[blk % 2]
            nT3 = nT.re("p (c t) -> p c t", t=512)
            for t in range(4):
                i2 = (blk * 4 + t) % 2
                norm_block(k, h[b0 + t * 128:b0 + (t + 1) * 128, :], hts[i2], junk, sss[i2], rstds[i2], nbs[i2],
                           wbc, epst, ident_b, nT3, (P[0], P[1]), t)
            if CK == 2:
                k.S.emit(st)
                return nc
            k.dma(nT_s[:, :, b0:b0 + 512], nT3, eng=POOL)
            k.dma(posi, pos[:, b0:b0 + 512].v(lambda a: a.partition_broadcast(128)))
            k.cp(posf, posi)
            k.ts(ang, posf, ropet[:, 0:1], ALU.mult)
            for (dst, shift) in ((sin2, 0.0), (cos2, float(np.pi / 2))):
                k.ts(kq, ang, shift, ALU.add, 1.0 / TWO_PI, ALU.mult)
                k.cp(kqi, kq)
                k.cp(kq, kqi)
                k.stt(t1, kq, -TWO_PI, ang, ALU.mult, ALU.add)
                if shift != 0.0:
                    k.ts(t1, t1, shift, ALU.add)
                k.act(dst, t1, AF.Sin)
            k.ts(sin2, sin2, ropet[:, 1:2], ALU.mult)
            pi = [2]

            def nextp():
                p = P[2 + (pi[0] % 6)]
                pi[0] += 1
                return p

            def proj(cols, M):
                p = nextp()
                for c in range(16):
                    k.mm(p[:M], wa3[:, c, cols:cols + M], nT3[:, c, :], start=(c == 0), stop=(c == 15))
                return p

            if CK == 3:
                k.S.emit(st)
                return nc
            for m in range(4):
                p = proj(m * 128, 128)
                stage_out(p, qn_s[m, :, b0:b0 + 512])
            if CK == 4:
                k.S.emit(st)
                return nc
            for (ca, cb, M, dsts) in ((512, 768, 128, (qr_s[0], qr_s[1])), (640, 896, 128, (qr_s[2], qr_s[3])),
                                      (1536, 1600, 64, (kr_s,))):
                pa = proj(ca, M)
                pb = proj(cb, M)
                k.tt(t1[:M], pa[:M], cos2[:M], ALU.mult)
                k.tt(t2[:M], pb[:M], sin2[:M], ALU.mult)
                s_ = stg[stg_i[0] % 4]
                stg_i[0] += 1
                k.tt(s_[:M], t1[:M], t2[:M], ALU.add)
                for di, dd in enumerate(dsts):
                    k.dma(dd[:, b0:b0 + 512], s_[di * 64:(di + 1) * 64], eng=POOL)
            if CK == 5:
                k.S.emit(st)
                return nc
            for r in range(4):
                p = proj(1024 + r * 128, 128)
                k.cp(ckv[:, r, :], p, eng=ACT)
                k.act(sq[:, r, :], p, AF.Square)
            p = nextp()
            for r in range(4):
                k.mm(p, ones_f, sq[:, r, :], start=(r == 0), stop=(r == 3))
            k.act(rs_bc, p, AF.Sqrt, bias=epst, scale=1.0 / 512)
            k.recip(rs_bc, rs_bc)
            for r in range(4):
                k.stt(ckvn[:, r, :], ckv[:, r, :], kvnwt[:, r:r + 1], rs_bc, ALU.mult, ALU.mult)
            if CK == 6:
                k.S.emit(st)
                return nc
            for m in range(4):
                p = nextp()
                for r in range(4):
                    k.mm(p, wuk3[:, r, m * 128:(m + 1) * 128], ckvn[:, r, :], start=(r == 0), stop=(r == 3))
                stage_out(p, kn_s[m, :, b0:b0 + 512])
            for t in range(4):
                p = nextp()
                for r in range(4):
                    k.mm(p, ckvn[:, r, t * 128:(t + 1) * 128], wuv3[:, r, :], start=(r == 0), stop=(r == 3))
                stage_out(p, v_s[b0 + t * 128:b0 + (t + 1) * 128, :], eng=DVE)
        k.release()
        if stop == "A":
            k.S.emit(st)
            return nc

        k.mark()
        scale = 192.0 ** -0.5
        kr = k.alloc(S, BF16, "kr")
        k.dma(kr[:64], kr_s)
        NQ = S // 512
        kns = [k.alloc(S, BF16, "kn%d" % i) for i in range(2)]
        vts = [k.alloc(S, BF16, "v%d" % i) for i in range(2)]
        qns = [k.alloc(S, BF16, "qn%d" % i) for i in range(2)]
        qrs = [k.alloc(S, BF16, "qr%d" % i) for i in range(2)]
        pTs = [[k.alloc(512, BF16, "pT%d_%d" % (i, u)) for u in range(2)] for i in range(3)]
        rsum = k.alloc(512, F32, "rsum")
        sacc = [[k.alloc(512, F32, "sacc%d_%d" % (i, u)) for u in range(2)] for i in range(2)]
        ostg = [k.alloc(512, BF16, "ostg%d" % i) for i in range(2)]
        for m in range(4):
            kn, vt, qn, qr = kns[m % 2], vts[m % 2], qns[m % 2], qrs[m % 2]
            k.dma(kn, kn_s[m])
            k.dma(qn, qn_s[m])
            k.dma(qr[:64], qr_s[m])
            v3 = vt.re("p (t c) -> p t c", c=128)
            for tq in range(0, NCH, 16):
                te = min(tq + 16, NCH)
                k.dma(v3[:, tq:te, :], v_s.v(lambda a: a.rearrange("(t p) c -> p t c", p=128)[:, tq:te, m * 128:(m + 1) * 128]))
            for qp in range(NQ // 2):
                q0s = (qp * 1024, qp * 1024 + 512)
                pos_ = (P[6], P[7])

                def qk(kt):
                    for u in range(2):
                        k.mm(P[(kt % 3) * 2 + u], kn[:, kt * 128:(kt + 1) * 128], qn[:, q0s[u]:q0s[u] + 512], start=True, stop=False)
                    for u in range(2):
                        k.mm(P[(kt % 3) * 2 + u], kr[:64, kt * 128:(kt + 1) * 128], qr[:64, q0s[u]:q0s[u] + 512], start=False, stop=True)

                qk(0)
                for kt in range(NCH):
                    if kt + 1 < NCH:
                        qk(kt + 1)
                    for u in range(2):
                        k.act(pTs[kt % 3][u], P[(kt % 3) * 2 + u], AF.Exp, scale=scale)
                    for u in range(2):
                        k.mm(pos_[u], v3[:, kt, :], pTs[kt % 3][u], start=(kt == 0), stop=(kt == NCH - 1))
                    for u in range(2):
                        sa = sacc[kt % 2][u]
                        if kt < 2:
                            k.cp(sa, pTs[kt % 3][u])
                        else:
                            k.tt(sa, sa, pTs[kt % 3][u], ALU.add)
                for u in range(2):
                    if NCH > 1:
                        k.tt(sacc[0][u], sacc[0][u], sacc[1][u], ALU.add)
                    psm = P[u]
                    k.mm(psm, ones_f, sacc[0][u])
                    k.recip(rsum, psm)
                    os_ = ostg[u]
                    k.tt(os_, pos_[u], rsum, ALU.mult)
                    k.dma(yT[m * 128:(m + 1) * 128, q0s[u]:q0s[u] + 512], os_, eng=POOL)
        k.release()
        if stop == "B":
            k.S.emit(st)
            return nc

        dt_all = k.alloc(NCH * 16, F32, "dt_all").re("p (g n) -> p g n", n=16)
        a_all = k.alloc(NCH * 16, F32, "a_all").re("p (g n) -> p g n", n=16)
        aneg = k.alloc(16, F32, "aneg")
        dtb_bc = k.alloc(16, F32, "dtb_bc")
        d_bc = k.alloc(8, F32, "d_bc")
        snw_bc = k.alloc(512, F32, "snw_bc")
        cwt = k.alloc(30, F32, "convw").re("p (c k) -> p c k", k=5)
        cbt = k.alloc(6, F32, "convb")
        scwt = k.alloc(12, F32, "scw").re("p (c k) -> p c k", k=3)
        k.dma(aneg, alog.v(lambda a: a.partition_broadcast(128)))
        k.act(aneg, aneg, AF.Exp)
        k.ts(aneg, aneg, -1.0, ALU.mult)
        k.dma(dtb_bc, dtb.v(lambda a: a.partition_broadcast(128)))
        k.dma(d_bc, dsk.v(lambda a: a.partition_broadcast(128)))
        k.dma(snw_bc, snw.v(lambda a: a.partition_broadcast(128)))
        k.dma(cwt, convw.re("p (c k) -> p c k", k=5))
        k.dma(cbt, convb)
        k.dma(scwt, scw.re("p (c k) -> p c k", k=3))
        onet = k.alloc(1, F32, "one")
        k.memset(onet, 1.0)

        k.mark()
        ws = k.alloc(16 * NS, BF16, "ws")
        ws3 = ws.re("p (c n) -> p c n", n=NS)
        wc = k.alloc(16 * NCV, BF16, "wc")
        wc3 = wc.re("p (c n) -> p c n", n=NCV)
        for c4 in range(4):
            k.dma(ws[:, c4 * 4 * NS:(c4 + 1) * 4 * NS], WS[:, c4 * 4 * NS:(c4 + 1) * 4 * NS], eng=POOL)
        for c4 in range(4):
            k.dma(wc[:, c4 * 4 * NCV:(c4 + 1) * 4 * NCV], WC[:, c4 * 4 * NCV:(c4 + 1) * 4 * NCV], eng=POOL)
        nTs = [k.alloc(16 * 512, BF16, "nTc%d" % i) for i in range(2)]
        fst = [k.alloc(512, F32, "fst%d" % i) for i in range(4)]
        fi = [0]
        ctmp = k.alloc(512, F32, "ctmp")
        dtr = k.alloc(16, F32, "dtr")

        def fstage(psrc, dst, func=None, eng=ACT):
            s_ = fst[fi[0] % 4]
            fi[0] += 1
            if func is None:
                k.cp(s_, psrc, eng=eng)
            else:
                k.act(s_, psrc, func)
            k.dma(dst, s_, eng=POOL)

        for blk in range(NB):
            b0 = blk * 512
            nT3 = nTs[blk % 2].re("p (c t) -> p c t", t=512)
            k.dma(nT3, nT_s[:, :, b0:b0 + 512])
            pi = [0]

            def nextp():
                p = P[pi[0] % 8]
                pi[0] += 1
                return p

            for t in range(4):
                g = blk * 4 + t
                p = nextp()
                for c in range(16):
                    k.mm(p, nT3[:, c, t * 128:(t + 1) * 128], ws3[:, c, 0:512], start=(c == 0), stop=(c == 15))
                fstage(p, zs_s[b0 + t * 128:b0 + (t + 1) * 128, :], func=AF.Silu)
                p = nextp()
                for c in range(16):
                    k.mm(p[:, 0:16], nT3[:, c, t * 128:(t + 1) * 128], ws3[:, c, 1280:1296], start=(c == 0), stop=(c == 15))
                k.tt(dtr, p[:, 0:16], dtb_bc, ALU.add)
                k.act(dtr, dtr, AF.Exp)
                k.act(dt_all[:, g, :], dtr, AF.Ln, bias=onet, scale=1.0)
                k.tt(a_all[:, g, :], dt_all[:, g, :], aneg, ALU.mult)
            for c6 in range(6):
                p = nextp()
                for c in range(16):
                    k.mm(p, ws3[:, c, 512 + c6 * 128:512 + (c6 + 1) * 128], nT3[:, c, :], start=(c == 0), stop=(c == 15))
                fstage(p, xbc_s[c6, :, b0:b0 + 512], eng=(ACT if c6 % 2 == 0 else DVE))
            for c4 in range(4):
                p = nextp()
                for c in range(16):
                    k.mm(p, wc3[:, c, c4 * 128:(c4 + 1) * 128], nT3[:, c, :], start=(c == 0), stop=(c == 15))
                fstage(p, scb_s[c4, :, b0:b0 + 512])
                p1 = nextp()
                for c in range(16):
                    k.mm(p1, wc3[:, c, 512 + c4 * 128:512 + (c4 + 1) * 128], nT3[:, c, :], start=(c == 0), stop=(c == 15))
                p2 = nextp()
                for c in range(16):
                    k.mm(p2, wc3[:, c, 1024 + c4 * 128:1024 + (c4 + 1) * 128], nT3[:, c, :], start=(c == 0), stop=(c == 15))
                k.cp(ctmp, p1, eng=ACT)
                s_ = fst[fi[0] % 4]
                fi[0] += 1
                k.tt(s_, ctmp, p2, ALU.mult)
                k.dma(sccx_s[c4, :, b0:b0 + 512], s_, eng=POOL)
        k.release()
        if stop == "C1":
            k.S.emit(st)
            return nc

        k.mark()
        xws = [k.alloc(6 * 516, F32, "xw%d" % i).re("p (c t) -> p c t", t=516) for i in range(2)]
        xc = k.alloc(4 * 512, F32, "xc").re("p (c t) -> p c t", t=512)
        cacc = k.alloc(512, F32, "cacc")
        BTb = k.alloc(512, BF16, "BTb")
        CTb = k.alloc(512, BF16, "CTb")
        sbw = k.alloc(4 * 512, F32, "sbw").re("p (c t) -> p c t", t=512)
        scxw = k.alloc(4 * 514, F32, "scxw").re("p (c t) -> p c t", t=514)
        cvo = [k.alloc(512, BF16, "cvo%d" % i) for i in range(2)]
        Hs = k.alloc(512, F32, "H")
        Hb = k.alloc(512, BF16, "Hb")
        ecs = k.alloc(8, F32, "ecs")
        dch = k.alloc(8, F32, "dch")
        tmc = k.alloc(8, F32, "tmc")
        dst_ = k.alloc(8, F32, "dst")
        xdt = k.alloc(512, BF16, "xdt")
        xdtd = k.alloc(512, BF16, "xdtd")
        btok = k.alloc(128, BF16, "btok")
        dx = k.alloc(512, F32, "dx")
        Bm = k.alloc(1024, F32, "Bm")
        decT = k.alloc(1024, F32, "decT")
        cbm = k.alloc(128, F32, "cbm")
        MT = k.alloc(1024, BF16, "MT")
        y1 = k.alloc(512, F32, "y1")
        ys = [k.alloc(512, F32, "y%d" % i) for i in range(2)]
        ytl = [k.alloc(512, F32, "ytl%d" % i) for i in range(2)]
        zsl = [k.alloc(512, F32, "zsl%d" % i) for i in range(2)]
        ssn = k.alloc(1, F32, "ssn")
        rsn = k.alloc(1, F32, "rsn")
        yn = k.alloc(512, F32, "yn")
        yjunk = k.alloc(512, F32, "yjunk")
        yob = [k.alloc(512, BF16, "yob%d" % i) for i in range(2)]
        P1b = P[1].v(lambda a: a.bitcast(BF16))

        for d in range(2):
            Mcum, Mseg, Mstat, Mtri = (MU, MU, MSL, MU) if d == 0 else (ML, ML, MSU, ML)
            k.memset(Hs, 0.0)
            k.memset(Hb, 0.0)
            blks = list(range(NB)) if d == 0 else list(range(NB - 1, -1, -1))
            for bi, blk in enumerate(blks):
                b0 = blk * 512
                xw = xws[bi % 2]
                if d == 0:
                    lo = max(b0 - 2, 0)
                    hi = min(b0 + 514, S)
                    if b0 - 2 < 0:
                        k.memset(xw[:, :, 0:2], 0.0)
                    if b0 + 514 > S:
                        k.memset(xw[:, :, 514:516], 0.0)
                    k.dma(xw[:, :, lo - (b0 - 2):hi - (b0 - 2)], xbc_s.v(lambda a: a.rearrange("c p t -> p c t")[:, :, lo:hi]))
                    for c6 in range(6):
                        k.ts(cacc, xw[:, c6, 0:512], cwt[:, c6, 0:1], ALU.mult)
                        for kk in range(1, 5):
                            k.stt(cacc, xw[:, c6, kk:kk + 512], cwt[:, c6, kk:kk + 1], cacc, ALU.mult, ALU.add)
                        o = xc[:, c6, :] if c6 < 4 else (BTb if c6 == 4 else CTb)
                        k.act(o, cacc, AF.Silu, bias=cbt[:, c6:c6 + 1])
                    k.dma(xcs_s[blk].re("p (c t) -> p c t", t=512), xc, eng=POOL)
                    k.dma(bt_s[blk], BTb, eng=POOL)
                    k.dma(ct_s[blk], CTb, eng=POOL)
                else:
                    k.dma(xc, xcs_s[blk].re("p (c t) -> p c t", t=512))
                    k.dma(BTb, bt_s[blk])
                    k.dma(CTb, ct_s[blk])
                if d == 0:
                    lo1 = max(b0 - 1, 0)
                    hi1 = min(b0 + 513, S)
                    if b0 - 1 < 0:
                        k.memset(scxw[:, :, 0:1], 0.0)
                    if b0 + 513 > S:
                        k.memset(scxw[:, :, 513:514], 0.0)
                    k.dma(scxw[:, :, lo1 - (b0 - 1):hi1 - (b0 - 1)], sccx_s.v(lambda a: a.rearrange("c p t -> p c t")[:, :, lo1:hi1]))
                    k.dma(sbw, scb_s.v(lambda a: a.rearrange("c p t -> p c t")[:, :, b0:b0 + 512]))
                    for c4 in range(4):
                        k.ts(cacc, scxw[:, c4, 0:512], scwt[:, c4, 0:1], ALU.mult)
                        for kk in range(1, 3):
                            k.stt(cacc, scxw[:, c4, kk:kk + 512], scwt[:, c4, kk:kk + 1], cacc, ALU.mult, ALU.add)
                        o = cvo[c4 % 2]
                        k.tt(o, cacc, sbw[:, c4, :], ALU.mult)
                        k.dma(yT[1024 + c4 * 128:1024 + (c4 + 1) * 128, b0:b0 + 512], o, eng=POOL)
                chunks = list(range(4)) if d == 0 else [3, 2, 1, 0]
                for ci, cc in enumerate(chunks):
                    g = blk * 4 + cc
                    c0 = cc * 128
                    r0 = g * 128
                    a_d = a_all[:, g, d * 8:(d + 1) * 8]
                    dt_d = dt_all[:, g, d * 8:(d + 1) * 8]
                    yv = ys[g % 2]
                    if d == 1:
                        k.dma(ytl[g % 2], yt_s[r0:r0 + 128, :])
                        k.dma(zsl[g % 2], zs_s[r0:r0 + 128, :])
                    for c4 in range(4):
                        k.tr(P[0][:, c4 * 128:(c4 + 1) * 128], xc[:, c4, c0:c0 + 128], ident_f)
                    k.tr(P1b[:, 0:128], BTb[:, c0:c0 + 128], ident_b)
                    k.mm(P[1][:, 256:264], Mcum, a_d)
                    k.mm(P[1][:, 264:272], ones_f, a_d)
                    k.act(ecs, P[1][:, 256:264], AF.Exp)
                    k.act(dch, P[1][:, 264:272], AF.Exp)
                    k.cp(tmc, P[1][:, 264:272])
                    k.tt(tmc, tmc, P[1][:, 256:264], ALU.subtract)
                    k.act(dst_, tmc, AF.Exp)
                    k.tt(xdt.re("p (h q) -> p h q", q=64), P[0].re("p (h q) -> p h q", q=64), dt_d.bc(2, [128, 8, 64]), ALU.mult)
                    k.tt(xdtd.re("p (h q) -> p h q", q=64), xdt.re("p (h q) -> p h q", q=64), dst_.bc(2, [128, 8, 64]), ALU.mult)
                    k.cp(btok, P1b[:, 0:128], eng=ACT)
                    if d == 0:
                        k.tt(dx.re("p (h q) -> p h q", q=64), P[0].re("p (h q) -> p h q", q=64), d_bc.bc(2, [128, 8, 64]), ALU.mult)
                    k.tt(Bm.re("p (h l) -> p h l", l=128), Mseg.bc(1, [128, 8, 128]), a_d.bc(2, [128, 8, 128]), ALU.mult)
                    k.mm(P[2], Mstat, Bm[:, 0:512])
                    k.mm(P[3], Mstat, Bm[:, 512:1024])
                    k.act(decT[:, 0:512], P[2], AF.Exp)
                    k.act(decT[:, 512:1024], P[3], AF.Exp)
                    k.mm(P[4][:, 0:128], BTb[:, c0:c0 + 128], CTb[:, c0:c0 + 128])
                    k.tt(cbm, P[4][:, 0:128], Mtri, ALU.mult)
                    k.tt(MT.re("p (h l) -> p h l", l=128), decT.re("p (h l) -> p h l", l=128), cbm.bc(1, [128, 8, 128]), ALU.mult)
                    for hh in range(8):
                        k.mm(P[5][:, hh * 64:(hh + 1) * 64], MT[:, hh * 128:(hh + 1) * 128], xdt[:, hh * 64:(hh + 1) * 64])
                    k.mm(P[6], CTb[:, c0:c0 + 128], Hb)
                    k.tt(y1.re("p (h q) -> p h q", q=64), P[6].re("p (h q) -> p h q", q=64), ecs.bc(2, [128, 8, 64]), ALU.mult)
                    k.tt(yv, y1, P[5], ALU.add)
                    if d == 0:
                        k.tt(yv, yv, dx, ALU.add)
                        k.dma(yt_s[r0:r0 + 128, :], yv, eng=POOL)
                    k.mm(P[7], btok, xdtd)
                    k.tt(Hs.re("p (h q) -> p h q", q=64), Hs.re("p (h q) -> p h q", q=64), dch.bc(2, [128, 8, 64]), ALU.mult)
                    k.tt(Hs, Hs, P[7], ALU.add)
                    k.cp(Hb, Hs, eng=ACT)
                    if d == 1:
                        k.tt(yv, yv, ytl[g % 2], ALU.add)
                        k.tt(yv, yv, zsl[g % 2], ALU.mult)
                        k.act(yjunk, yv, AF.Square, accum=ssn)
                        rmsnorm_rstd(k, ssn, rsn, 512, epst)
                        k.stt(yn, yv, rsn, snw_bc, ALU.mult, ALU.mult)
                        for c4 in range(4):
                            k.tr(P[0][:, c4 * 128:(c4 + 1) * 128], yn[:, c4 * 128:(c4 + 1) * 128], ident_f)
                        o = yob[g % 2]
                        k.cp(o, P[0], eng=ACT)
                        k.dma(yT[512:1024, r0:r0 + 128].v(lambda a: a.rearrange("(c p) t -> p c t", p=128)),
                              o.re("p (c t) -> p c t", t=128), eng=POOL)
            if d == 0:
                k.S.barrier()
        k.release()
        k.S.emit(st)
    return nc


def arr_w(W):
    K, n = W.shape
    return np.ascontiguousarray(W.reshape(K // 128, 128, n).transpose(1, 0, 2)).reshape(128, (K // 128) * n)


def mixer_consts():
    r = np.arange(128)[:, None]
    c = np.arange(128)[None, :]
    masks = np.concatenate([(r <= c), (r >= c), (r < c), (r > c)], axis=1).astype(np.float32)
    freq = (10000.0 ** (-np.arange(0, 64, 2, dtype=np.float32) / 64)).astype(np.float32)
    p = np.arange(128)
    ropec = np.stack([freq[p % 32], np.where((p % 64) < 32, -1.0, 1.0)], axis=1).astype(np.float32)
    return {"ident": np.eye(128, dtype=np.float32), "masks": masks, "ropec": ropec}


def mixer_weights(inp, l, j):
    W = inp["w_in"][l]
    hs = range(4 * j, 4 * j + 4)
    qn = [W[:, h * 192:h * 192 + 128] for h in hs]
    qr = [W[:, h * 192 + 128:h * 192 + 192] for h in hs]
    qrs = [np.concatenate([x[:, 32:64], x[:, 0:32]], axis=1) for x in qr]
    ckv = W[:, 3072:3584]
    krp = W[:, 3584:3648]
    krs = np.concatenate([krp[:, 32:64], krp[:, 0:32]], axis=1)
    WA = np.concatenate(qn + qr + qrs + [ckv, krp, krs], axis=1)
    z0, x0, d0 = 3648, 5696, 8768
    WS = np.concatenate([W[:, z0 + 512 * j:z0 + 512 * (j + 1)], W[:, x0 + 512 * j:x0 + 512 * (j + 1)],
                         W[:, x0 + 2048 + 128 * j:x0 + 2048 + 128 * (j + 1)],
                         W[:, x0 + 2560 + 128 * j:x0 + 2560 + 128 * (j + 1)],
                         W[:, d0 + 8 * j:d0 + 8 * (j + 1)], W[:, d0 + 32 + 8 * j:d0 + 32 + 8 * (j + 1)]], axis=1)
    s0 = 8832
    WC = np.concatenate([W[:, s0 + 2048 * i + 512 * j:s0 + 2048 * i + 512 * (j + 1)] for i in range(3)], axis=1)
    chans = np.concatenate([np.arange(512 * j, 512 * (j + 1)), 2048 + np.arange(128 * j, 128 * (j + 1)),
                            2560 + np.arange(128 * j, 128 * (j + 1))])
    cw = inp["ssm_conv_w"][l][:, chans]
    convw = np.ascontiguousarray(cw.T.reshape(6, 128, 5).transpose(1, 0, 2)).reshape(128, 30)
    convb = np.ascontiguousarray(inp["ssm_conv_b"][l][chans].reshape(6, 128).T)
    sw = inp["sconv_w"][l][:, 512 * j:512 * (j + 1)]
    scw = np.ascontiguousarray(sw.T.reshape(4, 128, 3).transpose(1, 0, 2)).reshape(128, 12)
    f = np.float32
    return {
        "anw": inp["attn_norm_w"][l][None, :].astype(f),
        "WA": arr_w(WA), "WS": arr_w(WS), "WC": arr_w(WC),
        "kvnw": np.ascontiguousarray(inp["kv_norm_w"][l].reshape(4, 128).T),
        "wuk": arr_w(inp["w_uk"][l][:, 512 * j:512 * (j + 1)]),
        "wuv": arr_w(inp["w_uv"][l][:, 512 * j:512 * (j + 1)]),
        "convw": convw, "convb": convb,
        "alog": np.concatenate([inp["ssm_A_log"][l][0, 8 * j:8 * j + 8], inp["ssm_A_log"][l][1, 8 * j:8 * j + 8]])[None, :],
        "dtb": np.concatenate([inp["ssm_dt_bias"][l][0, 8 * j:8 * j + 8], inp["ssm_dt_bias"][l][1, 8 * j:8 * j + 8]])[None, :],
        "dsk": inp["ssm_D"][l][None, 8 * j:8 * j + 8],
        "snw": inp["ssm_norm_w"][l][None, 512 * j:512 * (j + 1)],
        "scw": scw,
    }


NE = 32


def build_rest(T, stop=None):
    CK = int(os.environ.get('MK_CK2', '0'))
    NBLK = T // 512
    HT = min(1024, T)
    NH = T // HT
    TPH = HT // 128
    SPH = HT // 512
    nc = bass.Bass("TRN2", target_bir_lowering=False)
    with ExitStack() as st:
        k = KB(nc, st, arena_words=50176)
        X = lambda name, shape, dt=F32: k.dram(name, shape, dt, kind="ExternalInput")
        h = X("h", [T, D])
        yTa = X("yTa", [128, 48 * T], BF16)
        anw = X("anw", [1, D])
        fnw = X("fnw", [1, D])
        finw = X("finw", [1, D])
        WGs = X("WGs", [16, 128, 16 * 384])
        WBs = X("WBs", [16, 128, 48 * 128])
        WOa = X("WOa", [128, 16 * D])
        rw = X("rw", [128, 16 * 40])
        rb = X("rb", [1, 40])
        WGE = X("WGE", [NE, 4, 128, 16 * 128])
        WUE = X("WUE", [NE, 4, 128, 16 * 128])
        WDE = X("WDE", [NE, 4, 128, D])
        ident_in = X("ident", [128, 128])
        h_out = k.dram("h_out", [T, D], F32, kind="ExternalOutput")
        y_out = k.dram("y_out", [T, D], F32, kind="ExternalOutput")
        mT_s = k.dram("mT_s", [128, 16, T], BF16)
        hn_s = k.dram("hn_s", [T, D], F32)
        n2T_s = k.dram("n2T_s", [128, 16, T], BF16)
        P = k.ps
        pi = [0]

        def nextp():
            p = P[pi[0] % 8]
            pi[0] += 1
            return p

        ident_f = k.alloc(128, F32, "ident_f")
        ident_b = k.alloc(128, BF16, "ident_b")
        epst = k.alloc(1, F32, "eps")
        k.dma(ident_f, ident_in)
        k.dma(ident_b, ident_in, eng=POOL)
        k.memset(epst, EPS)
        comb_all = k.alloc((T // 128) * 32, F32, "comb").re("p (t e) -> p t e", e=32)
        yTa3 = yTa.re("p (k t) -> p k t", t=T)

        k.mark()
        wbc = k.alloc(D, F32, "anw_bc")
        k.dma(wbc, anw.v(lambda a: a.partition_broadcast(128)))
        hts = [k.alloc(D, F32, "ht%d" % i) for i in range(2)]
        junk = k.alloc(D, BF16, "junk")
        nbs = [k.alloc(D, BF16, "nb%d" % i) for i in range(2)]
        sss = [k.alloc(1, F32, "ss%d" % i) for i in range(2)]
        rstds = [k.alloc(1, F32, "rstd%d" % i) for i in range(2)]
        nT3 = k.alloc(16 * 512, BF16, "nT").re("p (c t) -> p c t", t=512)
        ytb = k.alloc(48 * 512, BF16, "ytb").re("p (k t) -> p k t", t=512)
        wgs = [k.alloc(16 * 384, BF16, "wg%d" % i).re("p (c n) -> p c n", n=384) for i in range(2)]
        wbs = [k.alloc(48 * 128, BF16, "wb%d" % i).re("p (k n) -> p k n", n=128) for i in range(2)]
        mTs = k.alloc(16 * 512, BF16, "mTs").re("p (m t) -> p m t", t=512)
        gts = [k.alloc(512, F32, "g%d" % i) for i in range(2)]
        macc = k.alloc(512, F32, "macc")
        tmp = k.alloc(512, F32, "tmp")
        for blk in range(NBLK):
            b0 = blk * 512
            for t in range(4):
                i2 = (blk * 4 + t) % 2
                norm_block(k, h[b0 + t * 128:b0 + (t + 1) * 128, :], hts[i2], junk, sss[i2], rstds[i2], nbs[i2],
                           wbc, epst, ident_b, nT3, (P[0], P[1]), t)
            for i in range(3):
                k.dma(ytb[:, i * 16:(i + 1) * 16, :], yTa3[:, i * 16:(i + 1) * 16, b0:b0 + 512])
            pi[0] = 2
            for m in range(16):
                wg = wgs[m % 2]
                wb = wbs[m % 2]
                k.dma(wg, WGs[m].re("p (c n) -> p c n", n=384), eng=POOL)
                k.dma(wb, WBs[m].re("p (k n) -> p k n", n=128), eng=POOL)
                for i in range(3):
                    pg = P[2 + (pi[0] % 6)]
                    pi[0] += 1
                    for c in range(16):
                        k.mm(pg, wg[:, c, i * 128:(i + 1) * 128], nT3[:, c, :], start=(c == 0), stop=(c == 15))
                    g = gts[i % 2]
                    k.act(g, pg, AF.Sigmoid)
                    py = P[2 + (pi[0] % 6)]
                    pi[0] += 1
                    for kk in range(16):
                        k.mm(py, wb[:, i * 16 + kk, :], ytb[:, i * 16 + kk, :], start=(kk == 0), stop=(kk == 15))
                    if i == 0:
                        k.tt(macc, g, py, ALU.mult)
                    elif i == 1:
                        k.tt(tmp, g, py, ALU.mult)
                        k.tt(macc, macc, tmp, ALU.add)
                    else:
                        k.tt(tmp, g, py, ALU.mult)
                        k.tt(mTs[:, m, :], macc, tmp, ALU.add)
            k.dma(mT_s[:, :, b0:b0 + 512], mTs, eng=POOL)
        k.release()
        if stop == "1a":
            k.S.emit(st)
            return nc

        k.mark()
        wo3 = k.alloc(16 * D, BF16, "wo").re("p (m n) -> p m n", n=D)
        for c4 in range(4):
            k.dma(wo3[:, c4 * 4:(c4 + 1) * 4, :], WOa.re("p (m n) -> p m n", n=D)[:, c4 * 4:(c4 + 1) * 4, :], eng=POOL)
        fbc = k.alloc(D, F32, "fnw_bc")
        k.dma(fbc, fnw.v(lambda a: a.partition_broadcast(128)))
        rwt = k.alloc(16 * 40, F32, "rw").re("p (c n) -> p c n", n=40)
        k.dma(rwt, rw.re("p (c n) -> p c n", n=40))
        rb_bc = k.alloc(40, F32, "rb_bc")
        k.dma(rb_bc, rb.v(lambda a: a.partition_broadcast(128)))
        mTb = [k.alloc(16 * 512, BF16, "mTb%d" % i).re("p (m t) -> p m t", t=512) for i in range(2)]
        hts = [k.alloc(D, F32, "htb%d" % i) for i in range(2)]
        junk = k.alloc(D, BF16, "junkb")
        n2 = k.alloc(D, F32, "n2")
        n2Tf = k.alloc(16 * 128, F32, "n2Tf").re("p (c t) -> p c t", t=128)
        n2Tb = [k.alloc(16 * 512, BF16, "n2Tb%d" % i).re("p (c t) -> p c t", t=512) for i in range(2)]
        ss = k.alloc(1, F32, "ssb")
        rstd = k.alloc(1, F32, "rstdb")
        lgt = k.alloc(40, F32, "lgt")
        sm = {nm: k.alloc(n_, F32, nm) for nm, n_ in (("gmax", 1), ("gsh", 8), ("gex", 8), ("gsum", 1), ("gw", 1),
                                                      ("oh", 8), ("sel", 32), ("eg", 4), ("m1", 1), ("mask1", 4),
                                                      ("eg2", 4), ("m2", 1), ("mask2", 4), ("dd", 1), ("w2", 1),
                                                      ("w1", 1), ("c1", 4), ("cg", 4))}
        if CK == 1:
            k.S.emit(st)
            return nc
        for blk in range(NBLK):
            b0 = blk * 512
            mb = mTb[blk % 2]
            k.dma(mb, mT_s[:, :, b0:b0 + 512])
            for t in range(4):
                gt = blk * 4 + t
                r0 = b0 + t * 128
                ht = hts[gt % 2]
                k.dma(ht, h[r0:r0 + 128, :])
                for n in range(4):
                    p = nextp()
                    for m in range(16):
                        k.mm(p, mb[:, m, t * 128:(t + 1) * 128], wo3[:, m, n * 512:(n + 1) * 512], start=(m == 0), stop=(m == 15))
                    k.tt(ht[:, n * 512:(n + 1) * 512], ht[:, n * 512:(n + 1) * 512], p, ALU.add)
                k.dma(hn_s[r0:r0 + 128, :], ht, eng=POOL)
                if CK == 2:
                    k.S.emit(st)
                    return nc
                k.act(junk, ht, AF.Square, accum=ss)
                rmsnorm_rstd(k, ss, rstd, D, epst)
                k.stt(n2, ht, rstd, fbc, ALU.mult, ALU.mult)
                nb_ = n2Tb[blk % 2]
                MV = int(os.environ.get('MK_V', '9'))
                for q in range(4 if MV >= 2 else 0):
                    bank = nextp()
                    for c in range(4):
                        k.tr(bank[:, c * 128:(c + 1) * 128], n2[:, (q * 4 + c) * 128:(q * 4 + c + 1) * 128], ident_f)
                    k.cp(n2Tf[:, q * 4:(q + 1) * 4, :], bank.re("p (c t) -> p c t", t=128), eng=ACT)
                    if MV >= 3:
                        k.cp(nb_[:, q * 4:(q + 1) * 4, t * 128:(t + 1) * 128], bank.re("p (c t) -> p c t", t=128), eng=DVE)
                if t == 3:
                    k.dma(n2T_s[:, :, b0:b0 + 512], nb_, eng=POOL)
                if CK == 3:
                    k.S.emit(st)
                    return nc
                p = nextp()
                for c in range(16):
                    k.mm(p[:, 0:40], n2Tf[:, c, :], rwt[:, c, :], start=(c == 0), stop=(c == 15))
                k.tt(lgt, p[:, 0:40], rb_bc, ALU.add)
                if CK == 4:
                    k.S.emit(st)
                    return nc
                lg = lgt[:, 0:8]
                le = lgt[:, 8:40]
                k.reduce(sm["gmax"], lg, ALU.max)
                k.ts(sm["gsh"], lg, sm["gmax"], ALU.subtract)
                k.act(sm["gex"], sm["gsh"], AF.Exp, accum=sm["gsum"])
                k.recip(sm["gw"], sm["gsum"])
                k.ts(sm["oh"], lg, sm["gmax"], ALU.is_equal)
                k.tt(sm["sel"].re("p (g e) -> p g e", e=4), le.re("p (g e) -> p g e", e=4), sm["oh"].bc(2, [128, 8, 4]), ALU.mult)
                k.reduce(sm["eg"], sm["sel"].re("p (g e) -> p e g", e=4), ALU.add)
                k.reduce(sm["m1"], sm["eg"], ALU.max)
                k.ts(sm["mask1"], sm["eg"], sm["m1"], ALU.is_equal)
                k.stt(sm["eg2"], sm["mask1"], -1e30, sm["eg"], ALU.mult, ALU.add)
                k.reduce(sm["m2"], sm["eg2"], ALU.max)
                k.ts(sm["mask2"], sm["eg2"], sm["m2"], ALU.is_equal)
                k.tt(sm["dd"], sm["m2"], sm["m1"], ALU.subtract)
                k.act(sm["w2"], sm["dd"], AF.Sigmoid)
                k.ts(sm["w1"], sm["w2"], -1.0, ALU.mult, 1.0, ALU.add)
                k.ts(sm["c1"], sm["mask1"], sm["w1"], ALU.mult)
                k.stt(sm["cg"], sm["mask2"], sm["w2"], sm["c1"], ALU.mult, ALU.add)
                k.ts(sm["cg"], sm["cg"], sm["gw"], ALU.mult)
                k.tt(comb_all[:, gt, :].re("p (g e) -> p g e", e=4), sm["oh"].bc(2, [128, 8, 4]), sm["cg"].bc(1, [128, 8, 4]), ALU.mult)
        k.release()
        if stop == "1b":
            k.S.emit(st)
            return nc

        k.mark()
        n2T = k.alloc(16 * HT, BF16, "n2T").re("p (c t) -> p c t", t=HT)
        accs = [k.alloc(D, F32, "acc%d" % i) for i in range(TPH)]
        gu_ring = [k.alloc(16 * 128, BF16, "gu%d" % i).re("p (c f) -> p c f", f=128) for i in range(12)]
        d_ring = [k.alloc(D, BF16, "dw%d" % i) for i in range(6)]
        actT = k.alloc(4 * HT, BF16, "actT").re("p (j t) -> p j t", t=HT)
        sgs = [k.alloc(512, F32, "sg%d" % i) for i in range(2)]
        fin_bc = k.alloc(D, F32, "finw_bc")
        k.dma(fin_bc, finw.v(lambda a: a.partition_broadcast(128)))
        junk2 = k.alloc(D, BF16, "junk2")
        ss2 = k.alloc(1, F32, "ss2")
        rs2 = k.alloc(1, F32, "rs2")
        gi = [0]
        di = [0]
        sgi = [0]
        for hf in range(NH):
            t0 = hf * HT
            k.dma(n2T, n2T_s[:, :, t0:t0 + HT])
            for t in range(TPH):
                k.dma(accs[t], hn_s[t0 + t * 128:t0 + (t + 1) * 128, :])
            for e in range(NE):
                for j in range(4):
                    wg_ = gu_ring[gi[0] % 12]
                    gi[0] += 1
                    wu_ = gu_ring[gi[0] % 12]
                    gi[0] += 1
                    k.dma(wg_, WGE[e, j].re("p (c f) -> p c f", f=128), eng=POOL)
                    k.dma(wu_, WUE[e, j].re("p (c f) -> p c f", f=128), eng=POOL)
                    for sub in range(SPH):
                        pg = nextp()
                        for c in range(16):
                            k.mm(pg, wg_[:, c, :], n2T[:, c, sub * 512:(sub + 1) * 512], start=(c == 0), stop=(c == 15))
                        pu = nextp()
                        for c in range(16):
                            k.mm(pu, wu_[:, c, :], n2T[:, c, sub * 512:(sub + 1) * 512], start=(c == 0), stop=(c == 15))
                        sg = sgs[sgi[0] % 2]
                        sgi[0] += 1
                        k.act(sg, pg, AF.Silu)
                        k.tt(actT[:, j, sub * 512:(sub + 1) * 512], sg, pu, ALU.mult)
                wd = []
                for j in range(4):
                    w_ = d_ring[di[0] % 6]
                    di[0] += 1
                    k.dma(w_, WDE[e, j], eng=POOL)
                    wd.append(w_)
                for t in range(TPH):
                    for n in range(4):
                        p = nextp()
                        for j in range(4):
                            k.mm(p, actT[:, j, t * 128:(t + 1) * 128], wd[j][:, n * 512:(n + 1) * 512], start=(j == 0), stop=(j == 3))
                        k.stt(accs[t][:, n * 512:(n + 1) * 512], p, comb_all[:, hf * TPH + t, e:e + 1],
                              accs[t][:, n * 512:(n + 1) * 512], ALU.mult, ALU.add)
            for t in range(TPH):
                r0 = t0 + t * 128
                k.dma(h_out[r0:r0 + 128, :], accs[t], eng=SP)
                k.act(junk2, accs[t], AF.Square, accum=ss2)
                rmsnorm_rstd(k, ss2, rs2, D, epst)
                k.stt(accs[t], accs[t], rs2, fin_bc, ALU.mult, ALU.mult)
                k.dma(y_out[r0:r0 + 128, :], accs[t], eng=SP)
        k.release()
        k.S.emit(st)
    return nc


def rest_weights(inp, l):
    f = np.float32
    W = inp["w_in"][l][:, 14976:21120]
    WGs = np.ascontiguousarray(W.reshape(16, 128, 3, 16, 128).transpose(3, 1, 0, 2, 4)).reshape(16, 128, 16 * 384)
    WB = inp["w_branch"][l]
    WBs = np.ascontiguousarray(WB.reshape(48, 128, 16, 128).transpose(2, 1, 0, 3)).reshape(16, 128, 48 * 128)
    rw = np.concatenate([inp["router_group_w"][l], inp["router_expert_w"][l]], axis=1)
    rb = np.concatenate([inp["router_group_b"][l], inp["router_expert_b"][l]])[None, :]
    eg = inp["expert_w_gate"][l]
    eu = inp["expert_w_up"][l]
    ed = inp["expert_w_down"][l]
    WGE = np.ascontiguousarray(eg.reshape(NE, 16, 128, 4, 128).transpose(0, 3, 2, 1, 4)).reshape(NE, 4, 128, 16 * 128)
    WUE = np.ascontiguousarray(eu.reshape(NE, 16, 128, 4, 128).transpose(0, 3, 2, 1, 4)).reshape(NE, 4, 128, 16 * 128)
    WDE = np.ascontiguousarray(ed.reshape(NE, 4, 128, D))
    return {
        "anw": inp["attn_norm_w"][l][None, :].astype(f), "fnw": inp["ffn_norm_w"][l][None, :].astype(f),
        "finw": inp["final_norm_w"][None, :].astype(f),
        "WGs": WGs, "WBs": WBs, "WOa": arr_w(inp["w_o"][l]), "rw": arr_w(rw), "rb": np.ascontiguousarray(rb),
        "WGE": WGE, "WUE": WUE, "WDE": WDE, "ident": np.eye(128, dtype=f),
    }


def arr_yT(yT_full, t0, T):
    blk = yT_full[:, t0:t0 + T]
    return np.ascontiguousarray(blk.reshape(48, 128, T).transpose(1, 0, 2)).reshape(128, 48 * T)


_NC_CACHE = {}


def kernel(**inputs):
    inp = {k_: np.asarray(v) for k_, v in inputs.items()}
    B, S, _ = inp["x"].shape
    L = inp["w_in"].shape[0]
    T = (B * S) // 8
    CPB = 8 // B
    if "mixer" not in _NC_CACHE:
        _NC_CACHE["mixer"] = build_mixer(S)
        _NC_CACHE["rest"] = build_rest(T)
    consts = mixer_consts()
    hcur = np.ascontiguousarray(inp["x"], dtype=np.float32)
    out = None
    for l in range(L):
        maps = []
        wj = [mixer_weights(inp, l, j) for j in range(4)]
        for core in range(8):
            b, j = core // 4, core % 4
            m = dict(consts)
            m.update(wj[j])
            m["h"] = hcur[b]
            m["pos"] = inp["positions"][b][None, :]
            maps.append({k_: np.ascontiguousarray(v) for k_, v in m.items()})
        res = run_bass_kernel_spmd(_NC_CACHE["mixer"], maps, core_ids=list(range(8)))
        del maps
        yT_b = []
        for b in range(B):
            parts = [np.asarray(res.results[b * 4 + j]["yT"]) for j in range(4)]
            yT_b.append(np.concatenate([p_[i * 512:(i + 1) * 512] for i in range(3) for p_ in parts], axis=0))
        rwts = rest_weights(inp, l)
        maps = []
        for core in range(8):
            b, q = core // CPB, core % CPB
            m = dict(rwts)
            m["h"] = np.ascontiguousarray(hcur[b, q * T:(q + 1) * T])
            m["yTa"] = arr_yT(yT_b[b], q * T, T)
            maps.append(m)
        res = run_bass_kernel_spmd(_NC_CACHE["rest"], maps, core_ids=list(range(8)))
        del maps, rwts
        hn = np.stack([np.concatenate([np.asarray(res.results[b * CPB + q]["h_out"]) for q in range(CPB)], axis=0) for b in range(B)])
        if l == L - 1:
            out = np.stack([np.concatenate([np.asarray(res.results[b * CPB + q]["y_out"]) for q in range(CPB)], axis=0) for b in range(B)])
        hcur = hn.astype(np.float32)
    return out.astype(np.float32)
```
